# Optimizing a Trainium2 kernel written in Bass

```python
import jax, jax.numpy as jnp
from jax import lax
import numpy as np

D_MODEL = 1024
BATCH = 8
SEQ = 2048
DEPTH = 2

N_EVEN = (DEPTH + 1) // 2
N_ODD = DEPTH // 2

CONV_DIM = D_MODEL // 2
CONV_WIDTH = 3
HGRN_HEADS = 4
HGRN_EXPAND = 128
HGRN_HEAD_V = (D_MODEL // 2) // HGRN_HEADS
HGRN_KEY_DIM = HGRN_HEADS * HGRN_EXPAND
HGRN_VAL_DIM = HGRN_HEADS * HGRN_HEAD_V
HGRN_CHUNK = 64
EVEN_SPLITS = [CONV_DIM] * 3 + [HGRN_KEY_DIM] * 2 + [HGRN_VAL_DIM] * 2
EVEN_IN = sum(EVEN_SPLITS)
EVEN_MIX = CONV_DIM + HGRN_VAL_DIM

RET_HEADS = 4
RET_HEAD_K = D_MODEL // RET_HEADS
RET_HEAD_V = 2 * RET_HEAD_K
RET_K_DIM = RET_HEADS * RET_HEAD_K
RET_V_DIM = RET_HEADS * RET_HEAD_V
RET_CHUNK = 128
ROPE_BASE = 10000.0
ODD_SPLITS = [RET_K_DIM, RET_K_DIM, RET_V_DIM, RET_V_DIM]
ODD_IN = sum(ODD_SPLITS)

FFN_HIDDEN = -(-(8 * D_MODEL) // (3 * 256)) * 256

EPS = 1e-6

kernel_name = "hybrid_conv_hgrn2_retention_trunk"


def _split_points(sizes):
    return [int(v) for v in np.cumsum(sizes)[:-1]]


def rmsnorm(x, w):
    xf = x.astype(jnp.float32)
    y = xf * lax.rsqrt(jnp.mean(xf * xf, axis=-1, keepdims=True) + EPS)
    return (y * w.astype(jnp.float32)).astype(x.dtype)


def short_conv_mixer(a, b, c, w_conv):
    S = a.shape[1]
    u = c * a
    up = jnp.pad(u, ((0, 0), (CONV_WIDTH - 1, 0), (0, 0)))
    y = up[:, 0:S] * w_conv[0]
    for k in range(1, CONV_WIDTH):
        y = y + up[:, k:k + S] * w_conv[k]
    return b * y


def hgrn2_mixer(q, f, i, g, lb, norm_w):
    Bn, S, _ = q.shape
    N = S // HGRN_CHUNK
    C = HGRN_CHUNK
    f32 = jnp.float32
    qf = jax.nn.silu(q.astype(f32))
    logf = jnp.logaddexp(jnp.log(lb), jnp.log1p(-lb) + jax.nn.log_sigmoid(f.astype(f32)))
    kf = -jnp.expm1(logf)
    vf = i.astype(f32)

    def to_chunks(t, hd):
        return t.reshape(Bn, N, C, HGRN_HEADS, hd).transpose(1, 0, 3, 2, 4)

    qc = to_chunks(qf, HGRN_EXPAND)
    kc = to_chunks(kf, HGRN_EXPAND)
    lc = to_chunks(logf, HGRN_EXPAND)
    vc = to_chunks(vf, HGRN_HEAD_V)
    causal = jnp.tril(jnp.ones((C, C), dtype=bool))

    def step(state, inp):
        qn, kn, ln, vn = inp
        b = jnp.cumsum(ln, axis=-2)
        o_inter = jnp.einsum('bhck,bhkv->bhcv', qn * jnp.exp(b), state)
        diff = b[:, :, :, None, :] - b[:, :, None, :, :]
        decay = jnp.exp(jnp.where(causal[:, :, None], diff, -jnp.inf))
        scores = jnp.einsum('bhik,bhijk,bhjk->bhij', qn, decay, kn)
        o_intra = jnp.einsum('bhij,bhjv->bhiv', scores, vn)
        b_last = b[:, :, -1:, :]
        new_state = (jnp.exp(b_last[:, :, 0, :])[..., None] * state
                     + jnp.einsum('bhck,bhcv->bhkv', kn * jnp.exp(b_last - b), vn))
        return new_state, o_inter + o_intra

    state0 = jnp.zeros((Bn, HGRN_HEADS, HGRN_EXPAND, HGRN_HEAD_V), f32)
    _, o = lax.scan(step, state0, (qc, kc, lc, vc))
    o = o.transpose(1, 0, 3, 2, 4).reshape(Bn, S, HGRN_HEADS, HGRN_HEAD_V)
    o = o * lax.rsqrt(jnp.mean(o * o, axis=-1, keepdims=True) + EPS)
    o = o * norm_w.astype(f32).reshape(HGRN_HEADS, HGRN_HEAD_V)
    o = o.reshape(Bn, S, HGRN_VAL_DIM) * jax.nn.silu(g.astype(f32))
    return o.astype(q.dtype)


def rotary(x, pos):
    half = x.shape[-1] // 2
    inv_freq = ROPE_BASE ** (-jnp.arange(half, dtype=jnp.float32) / half)
    ang = pos.astype(jnp.float32)[..., None] * inv_freq
    cos = jnp.cos(ang)[:, :, None, :]
    sin = jnp.sin(ang)[:, :, None, :]
    x1, x2 = x[..., :half], x[..., half:]
    return jnp.concatenate([x1 * cos - x2 * sin, x1 * sin + x2 * cos], axis=-1)


def retention_mixer(q, k, v, g, pos, norm_w):
    Bn, S, _ = q.shape
    C = RET_CHUNK
    N = S // C
    f32 = jnp.float32
    qh = rotary(q.astype(f32).reshape(Bn, S, RET_HEADS, RET_HEAD_K), pos)
    kh = rotary(k.astype(f32).reshape(Bn, S, RET_HEADS, RET_HEAD_K), pos) * (RET_HEAD_K ** -0.5)
    vh = v.astype(f32).reshape(Bn, S, RET_HEADS, RET_HEAD_V)
    log_gamma = jnp.log1p(-jnp.exp2(-5.0 - jnp.arange(RET_HEADS, dtype=f32)))
    idx = jnp.arange(C, dtype=f32)
    rel = idx[:, None] - idx[None, :]
    d_mat = jnp.where(rel >= 0, jnp.exp(log_gamma[:, None, None] * jnp.maximum(rel, 0.0)), 0.0)

    qc = qh.reshape(Bn, N, C, RET_HEADS, RET_HEAD_K)
    kc = kh.reshape(Bn, N, C, RET_HEADS, RET_HEAD_K)
    vc = vh.reshape(Bn, N, C, RET_HEADS, RET_HEAD_V)
    scores = jnp.einsum('bnihd,bnjhd->bnhij', qc, kc) * d_mat
    o_intra = jnp.einsum('bnhij,bnjhv->bnihv', scores, vc)

    q_dec = qc * jnp.exp(log_gamma[None, :] * (idx[:, None] + 1.0))[:, :, None]
    k_dec = kc * jnp.exp(log_gamma[None, :] * (C - 1.0 - idx[:, None]))[:, :, None]
    chunk_decay = jnp.exp(log_gamma * C)[None, :, None, None]

    def step(state, inp):
        qd, kd, vn = inp
        o = jnp.einsum('bchk,bhkv->bchv', qd, state)
        new_state = chunk_decay * state + jnp.einsum('bchk,bchv->bhkv', kd, vn)
        return new_state, o

    state0 = jnp.zeros((Bn, RET_HEADS, RET_HEAD_K, RET_HEAD_V), f32)
    _, o_inter = lax.scan(step, state0, (q_dec.transpose(1, 0, 2, 3, 4),
                                         k_dec.transpose(1, 0, 2, 3, 4),
                                         vc.transpose(1, 0, 2, 3, 4)))
    o = (o_intra + o_inter.transpose(1, 0, 2, 3, 4)).reshape(Bn, S, RET_HEADS, RET_HEAD_V)
    mu = jnp.mean(o, axis=-1, keepdims=True)
    oc = o - mu
    y = oc * lax.rsqrt(jnp.mean(oc * oc, axis=-1, keepdims=True) + EPS)
    y = y * norm_w.astype(f32).reshape(RET_HEADS, RET_HEAD_V)
    y = y.reshape(Bn, S, RET_V_DIM) * jax.nn.silu(g.astype(f32))
    return y.astype(q.dtype)


def setup_inputs(seed: int = 0) -> dict:
    key = jax.random.key(seed)
    ks = jax.random.split(key, 16)
    f32 = jnp.float32

    def dense(k, shape, fan_in):
        return jax.random.normal(k, shape, f32) * (fan_in ** -0.5)

    def gain(k, shape):
        return 1.0 + 0.02 * jax.random.normal(k, shape, f32)

    return {
        "x": jax.random.normal(ks[0], (BATCH, SEQ, D_MODEL), f32),
        "positions": jnp.broadcast_to(jnp.arange(SEQ, dtype=jnp.int32), (BATCH, SEQ)),
        "norm_mix_w": gain(ks[1], (DEPTH, D_MODEL)),
        "norm_ffn_w": gain(ks[2], (DEPTH, D_MODEL)),
        "norm_final_w": gain(ks[3], (D_MODEL,)),
        "even_w_in": dense(ks[4], (N_EVEN, D_MODEL, EVEN_IN), D_MODEL),
        "conv_w": dense(ks[5], (N_EVEN, CONV_WIDTH, CONV_DIM), CONV_WIDTH),
        "hgrn_lb_logits": 0.1 * jax.random.normal(ks[6], (N_EVEN + 1, HGRN_KEY_DIM), f32),
        "hgrn_norm_w": gain(ks[7], (N_EVEN, HGRN_VAL_DIM)),
        "even_w_out": dense(ks[8], (N_EVEN, EVEN_MIX, D_MODEL), EVEN_MIX),
        "odd_w_in": dense(ks[9], (N_ODD, D_MODEL, ODD_IN), D_MODEL),
        "ret_norm_w": gain(ks[10], (N_ODD, RET_V_DIM)),
        "odd_w_out": dense(ks[11], (N_ODD, RET_V_DIM, D_MODEL), RET_V_DIM),
        "ffn_w_in": dense(ks[12], (DEPTH, D_MODEL, 2 * FFN_HIDDEN), D_MODEL),
        "ffn_w_out": dense(ks[13], (DEPTH, FFN_HIDDEN, D_MODEL), FFN_HIDDEN),
    }


def reference(x, positions, norm_mix_w, norm_ffn_w, norm_final_w, even_w_in, conv_w,
              hgrn_lb_logits, hgrn_norm_w, even_w_out, odd_w_in, ret_norm_w, odd_w_out,
              ffn_w_in, ffn_w_out):
    lbs = jnp.cumsum(jax.nn.softmax(hgrn_lb_logits.astype(jnp.float32), axis=0), axis=0)[:N_EVEN]
    even_pts = _split_points(EVEN_SPLITS)
    odd_pts = _split_points(ODD_SPLITS)
    h = x
    for layer in range(DEPTH):
        hn = rmsnorm(h, norm_mix_w[layer])
        if layer % 2 == 0:
            e = layer // 2
            proj = hn @ even_w_in[e]
            a, b, c, q, f, i, g = jnp.split(proj, even_pts, axis=-1)
            y_conv = short_conv_mixer(a, b, c, conv_w[e])
            y_rec = hgrn2_mixer(q, f, i, g, lbs[e], hgrn_norm_w[e])
            mix = jnp.concatenate([y_conv, y_rec], axis=-1) @ even_w_out[e]
        else:
            o = layer // 2
            proj = hn @ odd_w_in[o]
            q, k, v, g = jnp.split(proj, odd_pts, axis=-1)
            mix = retention_mixer(q, k, v, g, positions, ret_norm_w[o]) @ odd_w_out[o]
        h = h + mix
        hn = rmsnorm(h, norm_ffn_w[layer])
        gate, up = jnp.split(hn @ ffn_w_in[layer], 2, axis=-1)
        h = h + (jax.nn.silu(gate) * up) @ ffn_w_out[layer]
    return rmsnorm(h, norm_final_w)
```

```python
import math
import numpy as np
import concourse.bass as bass
import concourse.mybir as mybir
from concourse.bass_utils import run_bass_kernel_spmd

F32 = mybir.dt.float32
BF16 = mybir.dt.bfloat16
I32 = mybir.dt.int32
AF = mybir.ActivationFunctionType
ALU = mybir.AluOpType
AX = mybir.AxisListType

D = 1024
KD = 8
FF = 2816
EVEN_IN = 3584
ODD_IN = 6144
EPS = 1e-6
TWO_PI = 2.0 * math.pi
CW1 = 6.28125
CW2 = TWO_PI - CW1


class Res:
    __slots__ = ("w", "r")

    def __init__(self):
        self.w = None
        self.r = {}


class Sched:
    def __init__(self, nc, n_dma_sems=24):
        self.nc = nc
        self.E = ["pe", "act", "dve", "pool", "sp"]
        self.sem = {}
        self.val = {}
        self.seen = {e: {} for e in self.E}
        self._cms = []
        for e in ["pe", "act", "dve", "pool"]:
            self.sem[e] = self._alloc("sem_" + e)
            self.val[e] = 0
        self.ring = {"sp": [], "pool": []}
        for q, n in (("sp", 8), ("pool", n_dma_sems - 8)):
            for i in range(n):
                k = "dma_%s%d" % (q, i)
                self.sem[k] = self._alloc("sem_" + k)
                self.val[k] = 0
                self.ring[q].append(k)
        self.ring_i = {"sp": 0, "pool": 0}
        self.prog = {e: [] for e in self.E}

    def _alloc(self, name):
        cm = self.nc.semaphore(name)
        h = cm.__enter__()
        self._cms.append(cm)
        return h

    def _wait(self, e, ev):
        key, val = ev
        if self.seen[e].get(key, 0) >= val:
            return
        if key == e and (e == "pe" or val > self.val[e]):
            return
        sem = self.sem[key]
        self.prog[e].append(lambda eng: eng.wait_ge(sem, val))
        self.seen[e][key] = val

    def _deps(self, e, reads, writes):
        for r in reads:
            if r.w is not None:
                self._wait(e, r.w)
        for w in writes:
            if w.w is not None:
                self._wait(e, w.w)
            for k, v in w.r.items():
                self._wait(e, (k, v))

    def _commit(self, ev, reads, writes):
        for r in reads:
            if r.r.get(ev[0], 0) < ev[1]:
                r.r[ev[0]] = ev[1]
        for w in writes:
            w.w = ev
            w.r = {}

    def op(self, e, fn, reads=(), writes=(), inc=True):
        self._deps(e, reads, writes)
        if inc:
            self.val[e] += 1
            sem = self.sem[e]
            self.prog[e].append(lambda eng: fn(eng).then_inc(sem, 1))
            ev = (e, self.val[e])
        else:
            self.prog[e].append(fn)
            ev = (e, self.val[e] + 1)
        self._commit(ev, reads, writes)

    def dma(self, q, out, in_, reads=(), writes=()):
        k = self.ring[q][self.ring_i[q]]
        self.ring_i[q] = (self.ring_i[q] + 1) % len(self.ring[q])
        if self.val[k]:
            self._wait(q, (k, self.val[k]))
        self._deps(q, reads, writes)
        sem = self.sem[k]
        self.prog[q].append(lambda eng: eng.dma_start(out=out, in_=in_).then_inc(sem, 16))
        self.val[k] += 16
        ev = (k, self.val[k])
        self._commit(ev, reads, writes)

    def fence(self, res_list):
        for e in self.E:
            for r in res_list:
                if r.w is not None:
                    self._wait(e, r.w)

    def barrier(self):
        for e in self.E:
            for k in ["pe", "act", "dve", "pool"]:
                if k != e and self.val[k]:
                    self._wait(e, (k, self.val[k]))

    def finish(self, q="sp"):
        for k in self.ring["sp"] + self.ring["pool"]:
            if self.val[k]:
                self._wait(q, (k, self.val[k]))
        prog = self.prog
        with self.nc.Block() as block:
            @block.sync
            def _(eng):
                for t in prog["sp"]:
                    t(eng)

            @block.tensor
            def _(eng):
                for t in prog["pe"]:
                    t(eng)

            @block.scalar
            def _(eng):
                for t in prog["act"]:
                    t(eng)

            @block.vector
            def _(eng):
                for t in prog["dve"]:
                    t(eng)

            @block.gpsimd
            def _(eng):
                for t in prog["pool"]:
                    t(eng)


class Arena:
    def __init__(self, nc, name, nelem, dt):
        self.t = nc.sbuf_tensor(name, [128, nelem], dt).__enter__()
        self.n = nelem
        self.off = 0
        self.top = nelem

    def reset(self):
        self.off = 0

    def alloc(self, n, parts=128):
        o = self.off
        n2 = (n + 15) // 16 * 16
        self.off += n2
        assert self.off <= self.top, (self.off, self.top)
        return self.t[0:parts, o:o + n]

    def alloc_top(self, n, parts=128):
        n2 = (n + 15) // 16 * 16
        self.top -= n2
        assert self.off <= self.top, (self.off, self.top)
        return self.t[0:parts, self.top:self.top + n]


def ffn_groups():
    return [[0, 1, 2], [3, 4, 5], [6, 7, 8], [9, 10]]


def weight_schedule(dr):
    L = []

    def in_tile(tag, w, pieces):
        wv = w.rearrange("(k p) c -> p k c", p=128)
        ps = []
        o = 0
        for (c0, n) in pieces:
            ps.append((o, o + n, wv[:, :, c0:c0 + n]))
            o += n
        L.append((tag, "in", ps))

    def out_tile(tag, w, r0):
        wv = w[r0:r0 + 256, :].rearrange("(r p) c -> p r c", p=128)
        L.append((tag, "out", [(0, 1024, wv)]))

    ew, eo, ow, oo = dr["even_w_in"], dr["even_w_out"], dr["odd_w_in"], dr["odd_w_out"]
    for cp in range(2):
        for i, nm in enumerate("abc"):
            in_tile((nm, cp), ew, [(i * 512 + cp * 256, 256)])
    out_tile(("eo", 0), eo, 0)
    out_tile(("eo", 1), eo, 256)
    for hh in range(4):
        in_tile(("qf", hh), ew, [(1536 + hh * 128, 128), (2048 + hh * 128, 128)])
    for i in range(2):
        in_tile(("hi", i), ew, [(2560 + i * 256, 256)])
    for i in range(2):
        in_tile(("hg", i), ew, [(3072 + i * 256, 256)])
    out_tile(("eo", 2), eo, 512)
    out_tile(("eo", 3), eo, 768)

    def ffn(l):
        wi, wo = dr["ffn_w_in"][l], dr["ffn_w_out"][l]
        for grp in ffn_groups():
            for jp in grp:
                in_tile(("ffg", l, jp), wi, [(jp * 256, 256)])
                in_tile(("ffu", l, jp), wi, [(FF + jp * 256, 256)])
            for jp in grp:
                out_tile(("ffo", l, jp), wo, jp * 256)

    ffn(0)
    for hh in range(4):
        in_tile(("rq", hh), ow, [(hh * 256, 256)])
        in_tile(("rk", hh), ow, [(1024 + hh * 256, 256)])
        in_tile(("rv", hh, 0), ow, [(2048 + hh * 512, 256)])
        in_tile(("rv", hh, 1), ow, [(2048 + hh * 512 + 256, 256)])
        in_tile(("rg", hh, 0), ow, [(4096 + hh * 512, 256)])
        in_tile(("rg", hh, 1), ow, [(4096 + hh * 512 + 256, 256)])
        out_tile(("ro", hh, 0), oo, hh * 512)
        out_tile(("ro", hh, 1), oo, hh * 512 + 256)
    ffn(1)
    return L


class WStream:
    def __init__(self, S, nc, lst, NS):
        self.S = S
        self.NS = NS
        self.lst = lst
        self.ring = nc.sbuf_tensor("wring", [128, NS, 2048], BF16).__enter__()
        self.res = [Res() for _ in range(NS)]
        self.issued = 0
        self.consumed = 0
        self.released = [False] * len(lst)

    def view(self, i):
        s = i % self.NS
        if self.lst[i][1] == "in":
            return self.ring[:, s, :].rearrange("p (k c) -> p k c", k=8)
        return self.ring[:, s, :].rearrange("p (r c) -> p r c", r=2)

    def pump(self):
        while self.issued < len(self.lst):
            i = self.issued
            if i >= self.NS and not self.released[i - self.NS]:
                break
            v = self.view(i)
            for (c0, c1, src) in self.lst[i][2]:
                self.S.dma("pool", v[:, :, c0:c1], src, writes=[self.res[i % self.NS]])
            self.issued += 1

    def next(self, tag):
        i = self.consumed
        assert self.lst[i][0] == tag, (self.lst[i][0], tag)
        self.pump()
        assert self.issued > i, ("weight slot not available", tag)
        self.consumed += 1
        return i, self.view(i), self.res[i % self.NS]

    def done(self, i):
        self.released[i] = True
        self.pump()


def build_program(SL, NS=8):
    T = SL // 128
    GS = min(512, SL)
    NG = SL // GS
    TPG = GS // 128
    NCH = SL // 64
    CPG = GS // 64
    nc = bass.Bass("TRN2", target_bir_lowering=False)
    dr = {}

    def din(name, shape, dt=F32):
        dr[name] = nc.dram_tensor(name, shape, dt, kind="ExternalInput").ap()

    din("x", [SL, D])
    din("positions", [1, SL], I32)
    din("norm_mix_w", [128, 2 * KD])
    din("norm_ffn_w", [128, 2 * KD])
    din("norm_final_w", [1, D])
    din("even_w_in", [D, EVEN_IN])
    din("conv_w", [128, 12])
    din("hgrn_lb_logits", [128, 8])
    din("hgrn_norm_w", [1, 512])
    din("even_w_out", [1024, D])
    din("odd_w_in", [D, ODD_IN])
    din("ret_norm_w", [1, 2048])
    din("odd_w_out", [2048, D])
    din("ffn_w_in", [2, D, 2 * FF])
    din("ffn_w_out", [2, FF, D])
    din("c_ident", [128, 128])
    din("c_mask", [128, 128])
    din("c_smask", [128, 512])
    din("c_invf", [128, 1])
    din("c_rmask", [128, 512])
    din("c_rvec", [128, 12])
    out_d = nc.dram_tensor("out", [SL, D], F32, kind="ExternalOutput").ap()
    rot_d = nc.dram_tensor("rot_scratch", [2, 128, SL], F32).ap()
    r_rot = [Res(), Res()]

    S = Sched(nc)

    def sbt(name, shape, dt):
        return nc.sbuf_tensor(name, shape, dt).__enter__()

    h = sbt("h", [128, T, D], F32)
    r_h = [Res() for _ in range(T)]
    hnT = sbt("hnT", [128, KD, SL], BF16)
    r_hn = [Res() for _ in range(T)]
    W = WStream(S, nc, weight_schedule(dr), NS)
    ident_f = sbt("ident_f", [128, 128], F32)
    ident = sbt("ident", [128, 128], BF16)
    r_id = Res()
    cmask = sbt("cmask", [128, 128], F32)
    r_cm = Res()
    smask = sbt("smask", [128, 512], F32)
    r_sm = Res()
    nmw = sbt("nmw", [128, 2, KD], F32)
    nfw = sbt("nfw", [128, 2, KD], F32)
    r_nw = Res()
    small = sbt("small", [128, 256], F32)
    r_small = Res()
    AR = Arena(nc, "arena", 17600, F32)

    class _FA:
        @staticmethod
        def alloc(n, parts=128):
            return AR.alloc(n, parts)

    class _BA:
        @staticmethod
        def alloc(n, parts=128):
            m = (n + 1) // 2
            return AR.alloc(m, parts).bitcast(BF16)[:, 0:n]

    FA = _FA
    BA = _BA

    pm = [nc.psum_tensor("pm%d" % i, [128, 512], F32).__enter__() for i in range(8)]
    r_pm = [Res() for _ in range(8)]
    ptr = [pm[6 + i][:, :].bitcast(BF16) for i in range(2)]
    r_ptr = [r_pm[6], r_pm[7]]
    cnt = {"pm": 0, "ptr": 0}

    def bank():
        i = cnt["pm"] % 6
        cnt["pm"] += 1
        return pm[i], r_pm[i]

    def tbank():
        i = cnt["ptr"] % 2
        cnt["ptr"] += 1
        return ptr[i], r_ptr[i]

    def MM(out, lhsT, rhs, start, stop, reads, writes, inc):
        S.op("pe", lambda e: e.matmul(out, lhsT=lhsT, rhs=rhs, start=start, stop=stop), reads, writes, inc)

    def PROJ(out, lhs_list, rhs_list, reads, writes):
        n = len(lhs_list)
        for i in range(n):
            MM(out, lhs_list[i], rhs_list[i], i == 0, i == n - 1, reads, writes, i == n - 1)

    def TR(out, in_, np_, reads, writes, inc=True):
        idn = ident[0:np_, 0:np_]
        S.op("pe", lambda e: e.transpose(out=out, in_=in_, identity=idn), list(reads) + [r_id], writes, inc)

    def ACT(out, in_, func, reads, writes, scale=1.0, accum_out=None, bias=None):
        if bias is not None:
            S.op("act", lambda e: e.activation(out=out, in_=in_, func=func, scale=scale, bias=bias), reads, writes)
        elif accum_out is None:
            S.op("act", lambda e: e.activation(out=out, in_=in_, func=func, scale=scale), reads, writes)
        else:
            S.op("act", lambda e: e.activation(out=out, in_=in_, func=func, scale=scale, accum_out=accum_out),
                 reads, writes)

    def TT(eng, out, in0, in1, op, reads, writes):
        S.op(eng, lambda e: e.tensor_tensor(out=out, in0=in0, in1=in1, op=op), reads, writes)

    def TS(eng, out, in0, s1, s2, op0, op1, reads, writes):
        if s2 is None:
            S.op(eng, lambda e: e.tensor_scalar(out=out, in0=in0, scalar1=s1, scalar2=None, op0=op0), reads, writes)
        else:
            S.op(eng, lambda e: e.tensor_scalar(out=out, in0=in0, scalar1=s1, scalar2=s2, op0=op0, op1=op1),
                 reads, writes)

    def STT(out, in0, scalar, in1, op0, op1, reads, writes):
        S.op("dve", lambda e: e.scalar_tensor_tensor(out=out, in0=in0, scalar=scalar, in1=in1, op0=op0, op1=op1),
             reads, writes)

    def CP(eng, out, in_, reads, writes):
        S.op(eng, lambda e: e.tensor_copy(out=out, in_=in_), reads, writes)

    def RECIP(out, in_, reads, writes):
        S.op("dve", lambda e: e.reciprocal(out=out, in_=in_), reads, writes)

    def phase():
        S.barrier()
        AR.reset()

    S.dma("sp", ident_f[:], dr["c_ident"], writes=[r_id])
    S.dma("sp", cmask[:], dr["c_mask"], writes=[r_cm])
    S.dma("sp", smask[:], dr["c_smask"], writes=[r_sm])
    S.dma("sp", nmw[:].rearrange("p l k -> p (l k)"), dr["norm_mix_w"], writes=[r_nw])
    S.dma("sp", nfw[:].rearrange("p l k -> p (l k)"), dr["norm_ffn_w"], writes=[r_nw])
    S.dma("sp", small[:, 0:12], dr["conv_w"], writes=[r_small])
    S.dma("sp", small[:, 16:24], dr["hgrn_lb_logits"], writes=[r_small])
    S.dma("sp", small[:, 32:33], dr["c_invf"], writes=[r_small])
    xv = dr["x"].rearrange("(t p) d -> p t d", p=128)
    for t in range(T):
        S.dma("sp", h[:, t, :], xv[:, t, :], writes=[r_h[t]])
    CP("dve", ident[:], ident_f[:], [r_id], [r_id])
    TT("dve", small[:, 36:40], small[:, 16:20], small[:, 20:24], ALU.subtract, [r_small], [r_small])
    ACT(small[:, 40:44], small[:, 36:40], AF.Sigmoid, [r_small], [r_small])
    ACT(small[:, 44:48], small[:, 36:40], AF.Sigmoid, [r_small], [r_small], scale=-1.0)
    TS("dve", small[:, 48:52], small[:, 44:48], -1.0, None, ALU.mult, None, [r_small], [r_small])
    LB, OML, NOML = 40, 44, 48
    S.op("dve", lambda e: e.memset(small[:, 60:61], EPS), [r_small], [r_small])
    eps_col = small[:, 60:61]

    junk = sbt("junk", [128, D], BF16)
    r_junk = Res()
    xn = [sbt("xn%d" % i, [128, D], BF16) for i in range(2)]
    r_xn = [Res(), Res()]
    nst = sbt("nst", [128, 64], F32)
    r_nst = [Res() for _ in range(T)]

    def norm_gen(t, wcol, final=None):
        rn = r_nst[t]
        ACT(junk[:], h[:, t, :], AF.Square, [r_h[t]], [r_junk, rn], accum_out=nst[:, t:t + 1])
        yield
        TS("dve", nst[:, 16 + t:17 + t], nst[:, t:t + 1], 1.0 / D, EPS, ALU.mult, ALU.add, [rn], [rn])
        yield
        ACT(nst[:, 16 + t:17 + t], nst[:, 16 + t:17 + t], AF.Sqrt, [rn], [rn])
        yield
        RECIP(nst[:, 32 + t:33 + t], nst[:, 16 + t:17 + t], [rn], [rn])
        if final is not None:
            o_, ro = final["ot"][t % 2], final["r_ot"][t % 2]
            STT(o_, h[:, t, :], nst[:, 32 + t:33 + t], final["fw"], ALU.mult, ALU.mult,
                [r_h[t], rn, final["r_fw"]], [ro])
            yield
            S.dma("sp", final["ov"][:, t, :], o_, reads=[ro])
            return
        yield
        x_, rx = xn[t % 2], r_xn[t % 2]
        ACT(x_[:], h[:, t, :], AF.Copy, [r_h[t], rn], [rx], scale=nst[:, 32 + t:33 + t])
        yield
        wb = wcol.unsqueeze(2).to_broadcast([128, KD, 128])
        pt, rpt = tbank()
        for k in range(KD):
            TR(pt[:, k * 128:(k + 1) * 128], x_[:, k * 128:(k + 1) * 128], 128, [rx], [rpt], inc=(k == KD - 1))
        yield
        TT("dve", hnT[:, :, t * 128:(t + 1) * 128], pt[:].rearrange("p (k c) -> p k c", k=KD), wb, ALU.mult,
           [rpt, r_nw], [r_hn[t]])

    class NormPipe:
        def __init__(self, wcol, final=None):
            self.wcol = wcol
            self.final = final
            self.q = []
            self.fresh = []

        def push(self, t):
            g_ = norm_gen(t, self.wcol, self.final)
            next(g_)
            self.fresh.append(g_)

        def tick(self):
            for g_ in list(self.q):
                try:
                    next(g_)
                except StopIteration:
                    self.q.remove(g_)
            self.q += self.fresh
            self.fresh = []

        def flush(self):
            while self.q or self.fresh:
                self.tick()

    def hn_reads(g):
        return r_hn[g * TPG:(g + 1) * TPG]

    def out_proj_add(lhs_fn, nk, rhs_fn, reads, after_tile=None):
        for t in range(T):
            for half in range(2):
                p, rp = bank()
                PROJ(p[:, :], [lhs_fn(j, t) for j in range(nk)], [rhs_fn(j, half) for j in range(nk)], reads(t), [rp])
                hs = h[:, t, half * 512:(half + 1) * 512]
                TT("dve", hs, p[:, :], hs, ALU.add, [rp, r_h[t]], [r_h[t]])
            if after_tile is not None:
                after_tile.tick()
                after_tile.push(t)
        if after_tile is not None:
            after_tile.flush()

    np0 = NormPipe(nmw[:, 0, :])
    for t in range(T):
        np0.tick()
        np0.push(t)
    np0.flush()
    phase()
    TC = min(512, SL)
    g_cos = AR.alloc_top(TC)
    g_sin = AR.alloc_top(TC)
    tA = AR.alloc_top(TC)
    tB = AR.alloc_top(TC)
    r_tab = Res()
    RT = [r_tab]
    pos_i = tB.bitcast(I32)
    rot_stores = []

    def table_piece(ci):
        cl = slice(ci * TC, (ci + 1) * TC)
        S.dma("sp", pos_i, dr["positions"][:, cl].partition_broadcast(128), writes=RT)
        CP("dve", tA, pos_i, RT, RT)
        TS("dve", tA, tA, small[:, 32:33], None, ALU.mult, None, RT + [r_small], RT)
        TS("dve", pos_i, tA, 1.0 / TWO_PI, None, ALU.mult, None, RT, RT)
        CP("dve", g_cos, pos_i, RT, RT)
        STT(tA, g_cos, -CW1, tA, ALU.mult, ALU.add, RT, RT)
        STT(tA, g_cos, -CW2, tA, ALU.mult, ALU.add, RT, RT)

        def wrap(buf):
            TS("dve", tB, buf, math.pi, -TWO_PI, ALU.is_gt, ALU.mult, RT, RT)
            TT("dve", buf, buf, tB, ALU.add, RT, RT)
            TS("dve", tB, buf, -math.pi, TWO_PI, ALU.is_lt, ALU.mult, RT, RT)
            TT("dve", buf, buf, tB, ALU.add, RT, RT)

        wrap(tA)
        ACT(g_sin, tA, AF.Sin, RT, RT)
        TS("dve", tA, tA, math.pi / 2, None, ALU.add, None, RT, RT)
        wrap(tA)
        ACT(g_cos, tA, AF.Sin, RT, RT)
        r0, r1 = Res(), Res()
        S.dma("sp", rot_d[0][:, cl], g_cos, reads=RT, writes=[r0])
        S.dma("sp", rot_d[1][:, cl], g_sin, reads=RT, writes=[r1])
        r_tab.r[r0.w[0]] = r0.w[1]
        r_tab.r[r1.w[0]] = r1.w[1]
        rot_stores.extend([r0, r1])

    n_pieces = SL // TC
    mixT = BA.alloc(4 * SL).rearrange("p (c s) -> p c s", c=4)
    r_mix = [Res() for _ in range(NG)]
    ub = [FA.alloc(SL + 2), FA.alloc(SL + 2)]
    r_ub = [Res(), Res()]
    a_sb = [FA.alloc(GS), FA.alloc(GS)]
    r_asb = [Res(), Res()]
    tcv = [FA.alloc(GS), FA.alloc(GS)]
    r_tcv = [Res(), Res()]
    for i in range(2):
        S.op("dve", lambda e, i=i: e.memset(ub[i][:, 0:2], 0.0), [], [r_ub[i]])
    it = 0
    for cp in range(2):
        ia, wa, ra = W.next(("a", cp))
        ib, wb_, rb = W.next(("b", cp))
        ic, wc, rc = W.next(("c", cp))
        for ci in range(2):
            cc = cp * 2 + ci
            u, ru = ub[cc % 2], r_ub[cc % 2]
            cs = slice(ci * 128, (ci + 1) * 128)
            for g in range(NG):
                gsl = slice(g * GS, (g + 1) * GS)
                rhs = [hnT[:, k, gsl] for k in range(KD)]
                pa, rpa = bank()
                PROJ(pa[:, 0:GS], [wa[:, k, cs] for k in range(KD)], rhs, [ra] + hn_reads(g), [rpa])
                pc, rpc = bank()
                PROJ(pc[:, 0:GS], [wc[:, k, cs] for k in range(KD)], rhs, [rc] + hn_reads(g), [rpc])
                pb, rpb = bank()
                PROJ(pb[:, 0:GS], [wb_[:, k, cs] for k in range(KD)], rhs, [rb] + hn_reads(g), [rpb])
                asb, rasb = a_sb[it % 2], r_asb[it % 2]
                tc_, rtc = tcv[it % 2], r_tcv[it % 2]
                it += 1
                ACT(asb[:, :], pa[:, 0:GS], AF.Copy, [rpa], [rasb])
                TT("dve", u[:, 2 + g * GS:2 + (g + 1) * GS], pc[:, 0:GS], asb[:, :], ALU.mult, [rpc, rasb], [ru])
                TS("dve", tc_[:, :], u[:, 2 + g * GS:2 + (g + 1) * GS], small[:, 8 + cc:9 + cc], None, ALU.mult, None,
                   [ru, r_small], [rtc])
                STT(tc_[:, :], u[:, 1 + g * GS:1 + (g + 1) * GS], small[:, 4 + cc:5 + cc], tc_[:, :], ALU.mult, ALU.add,
                    [ru, rtc, r_small], [rtc])
                STT(tc_[:, :], u[:, g * GS:(g + 1) * GS], small[:, cc:cc + 1], tc_[:, :], ALU.mult, ALU.add,
                    [ru, rtc, r_small], [rtc])
                TT("dve", mixT[:, cc, gsl], pb[:, 0:GS], tc_[:, :], ALU.mult, [rpb, rtc], [r_mix[g]])
            if cc < n_pieces:
                table_piece(cc)
        W.done(ia)
        W.done(ib)
        W.done(ic)
    i0, wo0, ro0 = W.next(("eo", 0))
    i1, wo1, ro1 = W.next(("eo", 1))
    wos = [wo0, wo1]
    out_proj_add(lambda j, t: mixT[:, j, t * 128:(t + 1) * 128], 4,
                 lambda j, half: wos[j // 2][:, j % 2, half * 512:(half + 1) * 512],
                 lambda t: [r_mix[t // TPG], ro0, ro1])
    W.done(i0)
    W.done(i1)

    for ci in range(4, n_pieces):
        table_piece(ci)
    S.fence(rot_stores)
    AR.top = AR.n
    phase()
    gwn = FA.alloc(512, parts=64)
    r_gwn = Res()
    S.dma("sp", gwn, dr["hgrn_norm_w"].partition_broadcast(64), writes=[r_gwn])
    qt = BA.alloc(4 * SL).rearrange("p (h s) -> p h s", h=4)
    kt = BA.alloc(4 * SL).rearrange("p (h s) -> p h s", h=4)
    r_qk = [[Res() for _ in range(NG)] for _ in range(4)]
    Eh = FA.alloc(4 * NCH).rearrange("p (h c) -> p h c", h=4)
    r_E = Res()
    markA = AR.off
    tmpA = [{n: FA.alloc(GS) for n in ["sg", "qs", "kk", "bb", "eb", "enb", "sqt"]} for _ in range(2)]
    r_tmpA = [{n: Res() for n in ["sg", "qs", "kk", "bb", "eb", "enb", "sqt"]} for _ in range(2)]
    qf_tiles = {}

    def stageA(it, hh, g):
        tm, rt = tmpA[it % 2], r_tmpA[it % 2]
        sg, qs, kk, bb, eb, enb, sqt = (tm[n] for n in ["sg", "qs", "kk", "bb", "eb", "enb", "sqt"])
        if g == 0:
            qf_tiles[hh] = W.next(("qf", hh))
        iqf, wqf, rqf = qf_tiles[hh]
        lbc = small[:, LB + hh:LB + hh + 1]
        omlc = small[:, OML + hh:OML + hh + 1]
        nomlc = small[:, NOML + hh:NOML + hh + 1]
        gsl = slice(g * GS, (g + 1) * GS)
        rhs = [hnT[:, k, gsl] for k in range(KD)]
        pq, rpq = bank()
        PROJ(pq[:, 0:GS], [wqf[:, k, 0:128] for k in range(KD)], rhs, [rqf] + hn_reads(g), [rpq])
        pf, rpf = bank()
        PROJ(pf[:, 0:GS], [wqf[:, k, 128:256] for k in range(KD)], rhs, [rqf] + hn_reads(g), [rpf])
        if g == NG - 1:
            W.done(iqf)
        ACT(sg[:, :], pf[:, 0:GS], AF.Sigmoid, [rpf], [rt["sg"]])
        ACT(sqt[:, :], pq[:, 0:GS], AF.Sigmoid, [rpq], [rt["sqt"]])
        TT("dve", qs[:, :], pq[:, 0:GS], sqt[:, :], ALU.mult, [rpq, rt["sqt"]], [rt["qs"]])
        TS("dve", kk[:, :], sg[:, :], nomlc, omlc, ALU.mult, ALU.add, [rt["sg"], r_small], [rt["kk"]])
        TS("dve", sg[:, :], sg[:, :], omlc, lbc, ALU.mult, ALU.add, [rt["sg"], r_small], [rt["sg"]])
        yield
        ACT(sg[:, :], sg[:, :], AF.Ln, [rt["sg"]], [rt["sg"]])
        S.op("dve", lambda e: e.tensor_tensor_scan(out=bb[:, :], data0=smask[:, 0:GS], data1=sg[:, :], initial=0.0,
                                                   op0=ALU.mult, op1=ALU.add),
             [rt["sg"], r_sm], [rt["bb"]])
        ACT(eb[:, :], bb[:, :], AF.Exp, [rt["bb"]], [rt["eb"]])
        ACT(enb[:, :], bb[:, :], AF.Exp, [rt["bb"]], [rt["enb"]], scale=-1.0)
        TT("dve", qt[:, hh, gsl], qs[:, :], eb[:, :], ALU.mult, [rt["qs"], rt["eb"]], [r_qk[hh][g]])
        TT("pool", kt[:, hh, gsl], kk[:, :], enb[:, :], ALU.mult, [rt["kk"], rt["enb"]], [r_qk[hh][g]])
        CP("dve", Eh[:, hh, g * CPG:(g + 1) * CPG], eb[:, :].rearrange("p (c j) -> p c j", j=64)[:, :, 63],
           [rt["eb"]], [r_E])
        yield

    gensA = [stageA(i, i // NG, i % NG) for i in range(4 * NG)]
    for i in range(4 * NG + 1):
        if i < 4 * NG:
            next(gensA[i])
        if i >= 1:
            next(gensA[i - 1])
    S.barrier()
    AR.off = markA

    whi = [W.next(("hi", i)) for i in range(2)]
    whg = [W.next(("hg", i)) for i in range(2)]
    weo = [W.next(("eo", 2 + i)) for i in range(2)]
    V_c = [BA.alloc(512, parts=64) for _ in range(2)]
    r_Vc = [Res(), Res()]
    gs_c = FA.alloc(512, parts=64)
    r_gsc = Res()
    GW_t = [BA.alloc(1024, parts=64).rearrange("p (c v) -> p c v", c=2) for _ in range(3)]
    r_GW = [[Res(), Res()] for _ in range(3)]
    o_t = [FA.alloc(1024, parts=64).rearrange("p (c v) -> p c v", c=2) for _ in range(2)]
    r_ot = [[Res(), Res()], [Res(), Res()]]
    ktc = [BA.alloc(512, parts=64) for _ in range(2)]
    r_ktc = [Res(), Res()]
    sTc = [BA.alloc(256, parts=64) for _ in range(2)]
    r_sTc = [Res(), Res()]
    U4 = FA.alloc(512).rearrange("p (h v) -> p h v", h=4)
    r_U4 = [Res() for _ in range(4)]
    st4 = [BA.alloc(512).rearrange("p (h v) -> p h v", h=4) for _ in range(2)]
    r_st4 = [[Res() for _ in range(4)] for _ in range(2)]
    hstt = [FA.alloc(32, parts=64) for _ in range(2)]
    r_hstt = [Res(), Res()]
    mixt = [BA.alloc(512).rearrange("p (h s) -> p h s", h=4) for _ in range(2)]
    r_mixt = [Res(), Res()]
    pkT = pm[6][0:64, :].bitcast(BF16)
    pyT = pm[7][:, :].bitcast(BF16)
    mask4 = cmask[0:64, 0:64].unsqueeze(1).to_broadcast([64, 4, 64])

    def chunk_gen(c):
        csl = slice(c * 64, (c + 1) * 64)
        g = c // CPG
        t = c // 2
        cj = c % 2
        lhs = [hnT[:, k, csl] for k in range(KD)]
        for i in range(2):
            PROJ(pm[0][0:64, i * 256:(i + 1) * 256], lhs, [whi[i][1][:, k, :] for k in range(KD)],
                 [whi[i][2], r_hn[t]], [r_pm[0]])
        Vc, rVc = V_c[c % 2], r_Vc[c % 2]
        ACT(Vc, pm[0][0:64, :], AF.Copy, [r_pm[0]], [rVc])
        for i in range(2):
            PROJ(pm[1][0:64, i * 256:(i + 1) * 256], lhs, [whg[i][1][:, k, :] for k in range(KD)],
                 [whg[i][2], r_hn[t]], [r_pm[1]])
        ACT(gs_c, pm[1][0:64, :], AF.Silu, [r_pm[1]], [r_gsc])
        GW, rGW = GW_t[t % 3], r_GW[t % 3]
        TT("pool", GW[:, cj, :], gs_c, gwn, ALU.mult, [r_gsc, r_gwn], [rGW[cj]])
        for hh in range(4):
            TR(pkT[:, hh * 128:(hh + 1) * 128], kt[:, hh, csl], 128, [r_qk[hh][g]], [r_pm[6]], inc=(hh == 3))
        kc, rkc = ktc[c % 2], r_ktc[c % 2]
        CP("dve", kc, pkT[:, 0:512], [r_pm[6]], [rkc])
        for hh in range(4):
            MM(pm[2][0:64, hh * 64:(hh + 1) * 64], kt[:, hh, csl], qt[:, hh, csl], True, True, [r_qk[hh][g]], [r_pm[2]],
               hh == 3)
        sT, rsT = sTc[c % 2], r_sTc[c % 2]
        TT("dve", sT.rearrange("p (h i) -> p h i", h=4), pm[2][0:64, 0:256].rearrange("p (h i) -> p h i", h=4), mask4,
           ALU.mult, [r_pm[2], r_cm], [rsT])
        yield
        for hh in range(4):
            hs_ = slice(hh * 128, (hh + 1) * 128)
            MM(pm[3][0:64, hs_], sT[:, hh * 64:(hh + 1) * 64], Vc[:, hs_], True, c == 0, [rsT, rVc], [r_pm[3]],
               c == 0 and hh == 3)
            if c > 0:
                MM(pm[3][0:64, hs_], qt[:, hh, csl], st4[(c - 1) % 2][:, hh, :], False, True,
                   [r_qk[hh][g], r_st4[(c - 1) % 2][hh]], [r_pm[3]], hh == 3)
        ot_, rot_ = o_t[t % 2], r_ot[t % 2]
        ACT(ot_[:, cj, :], pm[3][0:64, :], AF.Copy, [r_pm[3]], [rot_[cj]])
        if c < NCH - 1:
            for hh in range(4):
                hs_ = slice(hh * 128, (hh + 1) * 128)
                MM(pm[4][:, hs_], kc[:, hs_], Vc[:, hs_], True, True, [rkc, rVc], [r_pm[4]], hh == 3)
            for hh in range(4):
                hs_ = slice(hh * 128, (hh + 1) * 128)
                if c == 0:
                    CP("dve", U4[:, hh, :], pm[4][:, hs_], [r_pm[4]], [r_U4[hh]])
                else:
                    STT(U4[:, hh, :], U4[:, hh, :], Eh[:, hh, c - 1:c], pm[4][:, hs_], ALU.mult, ALU.add,
                        [r_U4[hh], r_E, r_pm[4]], [r_U4[hh]])
                TS("dve", st4[c % 2][:, hh, :], U4[:, hh, :], Eh[:, hh, c:c + 1], None, ALU.mult, None,
                   [r_U4[hh], r_E], [r_st4[c % 2][hh]])
        yield

    def tile_gen(t):
        ot_, rot_ = o_t[t % 2], r_ot[t % 2]
        GW, rGW = GW_t[t % 3], r_GW[t % 3]
        hs, rhs_ = hstt[t % 2], r_hstt[t % 2]
        for cj in range(2):
            for hh in range(4):
                ACT(junk[0:64, 0:128], ot_[:, cj, hh * 128:(hh + 1) * 128], AF.Square, [rot_[cj]], [r_junk, rhs_],
                    accum_out=hs[:, cj * 4 + hh:cj * 4 + hh + 1])
        TS("dve", hs[:, 8:16], hs[:, 0:8], 1.0 / 128, EPS, ALU.mult, ALU.add, [rhs_], [rhs_])
        ACT(hs[:, 8:16], hs[:, 8:16], AF.Sqrt, [rhs_], [rhs_])
        RECIP(hs[:, 16:24], hs[:, 8:16], [rhs_], [rhs_])
        yield
        o4 = ot_[:, :, :].rearrange("p c (h v) -> p (c h) v", h=4)
        rstd_b = hs[:, 16:24].unsqueeze(2).to_broadcast([64, 8, 128])
        TT("dve", o4, o4, rstd_b, ALU.mult, [rot_[0], rot_[1], rhs_], [rot_[0], rot_[1]])
        TT("pool", GW[:, :, :], ot_[:, :, :], GW[:, :, :], ALU.mult, [rot_[0], rot_[1], rGW[0], rGW[1]], [rGW[0], rGW[1]])
        yield
        for cj in range(2):
            for hh in range(4):
                TR(pyT[:, hh * 128 + cj * 64:hh * 128 + (cj + 1) * 64], GW[:, cj, hh * 128:(hh + 1) * 128], 64,
                   [rGW[cj]], [r_pm[7]], inc=(cj == 1 and hh == 3))
        mx, rmx = mixt[t % 2], r_mixt[t % 2]
        ACT(mx, pyT[:, 0:512].rearrange("p (h s) -> p h s", h=4), AF.Copy, [r_pm[7]], [rmx])
        yield
        for half in range(2):
            PROJ(pm[5][:, :], [mx[:, j, :] for j in range(4)],
                 [weo[j // 2][1][:, j % 2, half * 512:(half + 1) * 512] for j in range(4)],
                 [rmx, weo[0][2], weo[1][2]], [r_pm[5]])
            hsl = h[:, t, half * 512:(half + 1) * 512]
            TT("dve", hsl, pm[5][:, :], hsl, ALU.add, [r_pm[5], r_h[t]], [r_h[t]])
            yield

    npH = NormPipe(nfw[:, 0, :])
    cg = [chunk_gen(c) for c in range(NCH)]
    tg = [tile_gen(t) for t in range(T)]
    tg_calls = [0] * T

    def tstep(t, n):
        if 0 <= t < T:
            assert tg_calls[t] == n, (t, tg_calls[t], n)
            next(tg[t])
            tg_calls[t] += 1

    for i in range(NCH + 8):
        if i < NCH:
            next(cg[i])
        if 0 <= i - 1 < NCH:
            next(cg[i - 1])
        if i % 2 == 0:
            tstep((i - 2) // 2, 0)
            tstep((i - 4) // 2, 2)
            tstep((i - 6) // 2, 4)
        else:
            tstep((i - 3) // 2, 1)
            tstep((i - 5) // 2, 3)
    assert all(n == 5 for n in tg_calls), tg_calls
    for w_ in whi + whg + weo:
        W.done(w_[0])
    for t in range(T):
        npH.tick()
        npH.push(t)
    npH.flush()

    def ffn(l, tail_setup, tail_tile):
        phase()
        tail_setup()
        actT = [BA.alloc(6 * SL).rearrange("p (c s) -> p c s", c=6) for _ in range(2)]
        r_act = [[[Res() for _ in range(NG)] for _ in range(6)] for _ in range(2)]
        sgt = [FA.alloc(GS), FA.alloc(GS)]
        r_sgt = [Res(), Res()]
        it = 0
        for gi, grp in enumerate(ffn_groups()):
            aT, rA = actT[gi % 2], r_act[gi % 2]
            for li, jp in enumerate(grp):
                ig_, wg, rg = W.next(("ffg", l, jp))
                iu_, wu, ru = W.next(("ffu", l, jp))
                for ci in range(2):
                    jj = li * 2 + ci
                    cs = slice(ci * 128, (ci + 1) * 128)
                    for g in range(NG):
                        gsl = slice(g * GS, (g + 1) * GS)
                        rhs = [hnT[:, k, gsl] for k in range(KD)]
                        pg, rpg = bank()
                        PROJ(pg[:, 0:GS], [wg[:, k, cs] for k in range(KD)], rhs, [rg] + hn_reads(g), [rpg])
                        pu, rpu = bank()
                        PROJ(pu[:, 0:GS], [wu[:, k, cs] for k in range(KD)], rhs, [ru] + hn_reads(g), [rpu])
                        st_, rst = sgt[it % 2], r_sgt[it % 2]
                        it += 1
                        ACT(st_[:, :], pg[:, 0:GS], AF.Silu, [rpg], [rst])
                        TT("dve", aT[:, jj, gsl], pu[:, 0:GS], st_[:, :], ALU.mult, [rpu, rst], [rA[jj][g]])
                W.done(ig_)
                W.done(iu_)
            wts = [W.next(("ffo", l, jp)) for jp in grp]
            nk = 2 * len(grp)
            out_proj_add(lambda j, t: aT[:, j, t * 128:(t + 1) * 128], nk,
                         lambda j, half: wts[j // 2][1][:, j % 2, half * 512:(half + 1) * 512],
                         lambda t: [rA[j][t // TPG] for j in range(nk)] + [w_[2] for w_ in wts],
                         after_tile=(tail_tile if gi == len(ffn_groups()) - 1 else None))
            for w_ in wts:
                W.done(w_[0])

    rot_sb = {}

    def rot_prefetch():
        rot_sb["cos"] = AR.alloc_top(SL)
        rot_sb["sin"] = AR.alloc_top(SL)
        rot_sb["r"] = Res()
        S.dma("sp", rot_sb["cos"], rot_d[0], reads=rot_stores, writes=[rot_sb["r"]])
        S.dma("sp", rot_sb["sin"], rot_d[1], reads=rot_stores, writes=[rot_sb["r"]])

    ffn(0, rot_prefetch, NormPipe(nmw[:, 1, :]))

    phase()
    cosT, sinT, r_cs = rot_sb["cos"], rot_sb["sin"], rot_sb["r"]
    rmask = FA.alloc(512)
    rvec = FA.alloc(16)
    r_rc = Res()
    S.dma("sp", rmask, dr["c_rmask"], writes=[r_rc])
    S.dma("sp", rvec[:, 0:12], dr["c_rvec"], writes=[r_rc])
    rnw = FA.alloc(512)
    r_rnw = Res()
    qT = BA.alloc(2 * SL).rearrange("p (a s) -> p a s", a=2)
    kT = BA.alloc(2 * SL).rearrange("p (a s) -> p a s", a=2)
    r_q = [Res() for _ in range(NG)]
    r_k = [Res() for _ in range(NG)]
    HG = GS // 2
    tt = [[FA.alloc(HG) for _ in range(4)] for _ in range(2)]
    r_tt = [[Res() for _ in range(4)] for _ in range(2)]
    V_sb = [BA.alloc(512), BA.alloc(512)]
    r_V = [Res(), Res()]
    gsb = [FA.alloc(512), FA.alloc(512)]
    r_gs = [Res(), Res()]
    ki_sb = [BA.alloc(256), BA.alloc(256)]
    r_ki = [Res(), Res()]
    sT2 = [BA.alloc(128), BA.alloc(128)]
    r_sT2 = [Res(), Res()]
    Ur = FA.alloc(1024).rearrange("p (a v) -> p a v", a=2)
    r_Ur = Res()
    stb = [BA.alloc(1024).rearrange("p (a v) -> p a v", a=2) for _ in range(2)]
    r_stb2 = [Res(), Res()]
    bnst = [FA.alloc(16), FA.alloc(16)]
    r_bn = [Res(), Res()]
    y1 = FA.alloc(512)
    r_y1 = Res()
    y_sb = [BA.alloc(512) for _ in range(3)]
    r_ysb = [Res() for _ in range(3)]
    yT = [BA.alloc(512).rearrange("p (j c) -> p j c", j=4) for _ in range(2)]
    r_yT = [Res(), Res()]
    rot_it = [0]
    hb_ps = pm[6][:, 0:128]
    hb_pt2 = pm[6][:, 256:512].bitcast(BF16)
    hb_pt = pm[7][:, 0:128].bitcast(BF16)
    r_hb = [r_pm[6], r_pm[7], r_pm[6]]
    def ret_stageA(hh):
        for (tag, dst, rdst) in (("rq", qT, r_q), ("rk", kT, r_k)):
            iw, ww, rw = W.next((tag, hh))
            for g in range(NG):
                gsl = slice(g * GS, (g + 1) * GS)
                rhs = [hnT[:, k, gsl] for k in range(KD)]
                pA, rpA = bank()
                PROJ(pA[:, 0:GS], [ww[:, k, 0:128] for k in range(KD)], rhs, [rw] + hn_reads(g), [rpA])
                pB, rpB = bank()
                PROJ(pB[:, 0:GS], [ww[:, k, 128:256] for k in range(KD)], rhs, [rw] + hn_reads(g), [rpB])
                for hf in range(2):
                    cl = slice(hf * HG, (hf + 1) * HG)
                    al = slice(g * GS + hf * HG, g * GS + (hf + 1) * HG)
                    t_, rt_ = tt[rot_it[0] % 2], r_tt[rot_it[0] % 2]
                    rot_it[0] += 1
                    TT("dve", t_[0], pA[:, cl], cosT[:, al], ALU.mult, [rpA, r_cs], [rt_[0]])
                    TT("dve", t_[1], pB[:, cl], sinT[:, al], ALU.mult, [rpB, r_cs], [rt_[1]])
                    TT("dve", t_[2], pA[:, cl], sinT[:, al], ALU.mult, [rpA, r_cs], [rt_[2]])
                    TT("dve", t_[3], pB[:, cl], cosT[:, al], ALU.mult, [rpB, r_cs], [rt_[3]])
                    TT("pool", dst[:, 0, al], t_[0], t_[1], ALU.subtract, [rt_[0], rt_[1]], [rdst[g]])
                    TT("pool", dst[:, 1, al], t_[2], t_[3], ALU.add, [rt_[2], rt_[3]], [rdst[g]])
                yield
            W.done(iw)

    def exhaust(gen_):
        if gen_ is not None:
            for _ in gen_:
                pass

    exhaust(ret_stageA(0))
    for hh in range(4):
        gC = (1.0 - 2.0 ** (-5 - hh)) ** 128
        S.dma("sp", rnw, dr["ret_norm_w"][:, hh * 512:(hh + 1) * 512].partition_broadcast(128), writes=[r_rnw])
        mask_h = rmask[:, hh * 128:(hh + 1) * 128]
        c_h = rvec[:, hh:hh + 1]
        c2_h = rvec[:, 4 + hh:5 + hh]
        dk_h = rvec[:, 8 + hh:9 + hh]
        wv = [W.next(("rv", hh, i)) for i in range(2)]
        wg_ = [W.next(("rg", hh, i)) for i in range(2)]
        wo_ = [W.next(("ro", hh, i)) for i in range(2)]

        def ret_tile(t):
            tsl = slice(t * 128, (t + 1) * 128)
            g = t // TPG
            lhs = [hnT[:, k, tsl] for k in range(KD)]
            pv, rpv = pm[0], r_pm[0]
            for i in range(2):
                PROJ(pv[:, i * 256:(i + 1) * 256], lhs, [wv[i][1][:, k, :] for k in range(KD)], [wv[i][2], r_hn[t]], [rpv])
            V, rV = V_sb[t % 2], r_V[t % 2]
            ACT(V, pv[:, :], AF.Copy, [rpv], [rV])
            pg, rpg = pm[1], r_pm[1]
            for i in range(2):
                PROJ(pg[:, i * 256:(i + 1) * 256], lhs, [wg_[i][1][:, k, :] for k in range(KD)], [wg_[i][2], r_hn[t]], [rpg])
            gs_, rgs = gsb[t % 2], r_gs[t % 2]
            ACT(gs_, pg[:, :], AF.Silu, [rpg], [rgs])
            TT("pool", gs_, gs_, rnw, ALU.mult, [rgs, r_rnw], [rgs])
            pt, rpt = hb_pt, r_hb[1]
            for a in range(2):
                TR(pt[:, a * 128:(a + 1) * 128], kT[:, a, tsl], 128, [r_k[g]], [rpt], inc=(a == 1))
            ki, rki = ki_sb[t % 2], r_ki[t % 2]
            TS("dve", ki, pt[:, 0:256], dk_h, None, ALU.mult, None, [rpt, r_rc], [rki])
            ps_, rps = hb_ps, r_hb[0]
            PROJ(ps_[:, 0:128], [kT[:, a, tsl] for a in range(2)], [qT[:, a, tsl] for a in range(2)],
                 [r_k[g], r_q[g]], [rps])
            sT, rsT = sT2[t % 2], r_sT2[t % 2]
            TT("dve", sT, ps_[:, 0:128], mask_h, ALU.mult, [rps, r_rc], [rsT])
            yield
            po, rpo = pm[2], r_pm[2]
            MM(po[:, :], sT, V, True, t == 0, [rsT, rV], [rpo], t == 0)
            if t > 0:
                sb_, rsb = stb[(t - 1) % 2], r_stb2[(t - 1) % 2]
                for a in range(2):
                    MM(po[:, :], qT[:, a, tsl], sb_[:, a, :], False, a == 1, [r_q[g], rsb], [rpo], a == 1)
            pkvs = []
            if t < T - 1:
                for a in range(2):
                    pkv, rpkv = pm[3 + a], r_pm[3 + a]
                    MM(pkv[:, :], ki[:, a * 128:(a + 1) * 128], V, True, True, [rki, rV], [rpkv], True)
                    pkvs.append((pkv, rpkv))
            bn, rbn = bnst[t % 2], r_bn[t % 2]
            S.op("dve", lambda e: e.bn_stats(out=bn[:, 0:6], in_=po[:, :]), [rpo], [rbn])
            S.op("dve", lambda e: e.bn_aggr(out=bn[:, 8:10], in_=bn[:, 0:6]), [rbn], [rbn])
            TS("dve", bn[:, 10:11], bn[:, 9:10], c2_h, EPS, ALU.mult, ALU.add, [rbn, r_rc], [rbn])
            ACT(bn[:, 10:11], bn[:, 10:11], AF.Sqrt, [rbn], [rbn])
            for a, (pkv, rpkv) in enumerate(pkvs):
                if t == 0:
                    CP("dve", Ur[:, a, :], pkv[:, :], [rpkv], [r_Ur])
                else:
                    STT(Ur[:, a, :], Ur[:, a, :], gC, pkv[:, :], ALU.mult, ALU.add, [r_Ur, rpkv], [r_Ur])
            if pkvs:
                TS("pool", stb[t % 2][:, :, :], Ur[:, :, :], gC, 1.0, ALU.mult, ALU.mult, [r_Ur], [r_stb2[t % 2]])
            RECIP(bn[:, 11:12], bn[:, 10:11], [rbn], [rbn])
            TS("dve", bn[:, 11:12], bn[:, 11:12], c_h, None, ALU.mult, None, [rbn, r_rc], [rbn])
            TS("dve", y1, po[:, :], bn[:, 8:9], bn[:, 11:12], ALU.subtract, ALU.mult, [rpo, rbn], [r_y1])
            ys, rys = y_sb[t % 3], r_ysb[t % 3]
            TT("pool", ys, y1, gs_, ALU.mult, [r_y1, rgs], [rys])
            yield
            pt2, rpt2 = hb_pt2, r_hb[2]
            for j in range(4):
                TR(pt2[:, j * 128:(j + 1) * 128], ys[:, j * 128:(j + 1) * 128], 128, [rys], [rpt2], inc=(j == 3))
            yT_, ryT = yT[t % 2], r_yT[t % 2]
            ACT(yT_, pt2[:, 0:512].rearrange("p (j c) -> p j c", j=4), AF.Copy, [rpt2], [ryT])
            yield
            for half in range(2):
                p, rp = pm[5], r_pm[5]
                PROJ(p[:, :], [yT_[:, j, :] for j in range(4)],
                     [wo_[j // 2][1][:, j % 2, half * 512:(half + 1) * 512] for j in range(4)],
                     [ryT, wo_[0][2], wo_[1][2]], [rp])
                hs = h[:, t, half * 512:(half + 1) * 512]
                TT("dve", hs, p[:, :], hs, ALU.add, [rp, r_h[t]], [r_h[t]])
                yield

        npR = NormPipe(nfw[:, 1, :])
        gens = [ret_tile(t) for t in range(T)]
        stage_of = [0] * T
        next_sa = ret_stageA(hh + 1) if hh < 3 else None
        for i in range(T + 3):
            for (lag, st_) in ((3, 2), (0, 0), (3, 3), (1, 1), (3, 4)):
                t = i - lag
                if 0 <= t < T:
                    assert stage_of[t] == st_, (t, stage_of[t], st_)
                    next(gens[t])
                    stage_of[t] += 1
            if next_sa is not None and i >= T:
                for _ in range(3):
                    next(next_sa, None)
        exhaust(next_sa)
        for w_ in wv + wg_ + wo_:
            W.done(w_[0])
    for t in range(T):
        npR.tick()
        npR.push(t)
    npR.flush()
    AR.top = AR.n

    fin = {}
    ov = out_d.rearrange("(t p) d -> p t d", p=128)

    fin["ov"] = ov

    def final_setup():
        fin["fw"] = FA.alloc(D)
        fin["r_fw"] = Res()
        S.dma("sp", fin["fw"], dr["norm_final_w"].partition_broadcast(128), writes=[fin["r_fw"]])
        fin["ot"] = [FA.alloc(D), FA.alloc(D)]
        fin["r_ot"] = [Res(), Res()]

    ffn(1, final_setup, NormPipe(None, final=fin))
    assert W.consumed == len(W.lst)
    S.finish("sp")
    return nc


def make_consts():
    c = {}
    c["c_ident"] = np.eye(128, dtype=np.float32)
    j = np.arange(128)
    c["c_mask"] = (j[:, None] <= j[None, :]).astype(np.float32)
    sm = np.ones((128, 512), dtype=np.float32)
    sm[:, ::64] = 0.0
    c["c_smask"] = sm
    c["c_invf"] = (10000.0 ** (-np.arange(128, dtype=np.float64) / 128)).astype(np.float32).reshape(128, 1)
    rm = np.zeros((128, 4, 128))
    rv = np.zeros((128, 12))
    for hh in range(4):
        gam = 1.0 - 2.0 ** (-5 - hh)
        kinv = gam ** (-(j + 1.0)) * (256 ** -0.5)
        rm[:, hh, :] = (j[:, None] <= j[None, :]) * kinv[:, None]
        rv[:, hh] = gam ** (j + 1.0)
        rv[:, 4 + hh] = gam ** (2 * (j + 1.0))
        rv[:, 8 + hh] = kinv
    c["c_rmask"] = rm.reshape(128, 512).astype(np.float32)
    c["c_rvec"] = rv.astype(np.float32)
    return c


def make_in_maps(inputs, n, SL):
    f = lambda a: np.ascontiguousarray(np.asarray(a))
    shared = {
        "norm_mix_w": f(f(inputs["norm_mix_w"]).reshape(2, KD, 128).transpose(2, 0, 1)).reshape(128, 2 * KD),
        "norm_ffn_w": f(f(inputs["norm_ffn_w"]).reshape(2, KD, 128).transpose(2, 0, 1)).reshape(128, 2 * KD),
        "norm_final_w": f(inputs["norm_final_w"]).reshape(1, D),
        "even_w_in": f(inputs["even_w_in"])[0],
        "conv_w": f(f(inputs["conv_w"])[0].reshape(3, 4, 128).transpose(2, 0, 1)).reshape(128, 12),
        "hgrn_lb_logits": f(f(inputs["hgrn_lb_logits"]).reshape(2, 4, 128).transpose(2, 0, 1)).reshape(128, 8),
        "hgrn_norm_w": f(inputs["hgrn_norm_w"]).reshape(1, 512),
        "even_w_out": f(inputs["even_w_out"])[0],
        "odd_w_in": f(inputs["odd_w_in"])[0],
        "ret_norm_w": f(inputs["ret_norm_w"]).reshape(1, 2048),
        "odd_w_out": f(inputs["odd_w_out"])[0],
        "ffn_w_in": f(inputs["ffn_w_in"]),
        "ffn_w_out": f(inputs["ffn_w_out"]),
    }
    shared.update(make_consts())
    x = f(inputs["x"])
    pos = f(inputs["positions"]).astype(np.int32)
    maps = []
    for b in range(n):
        m = dict(shared)
        m["x"] = np.ascontiguousarray(x[b])
        m["positions"] = np.ascontiguousarray(pos[b].reshape(1, SL))
        maps.append(m)
    return maps


_CACHE = {}


def kernel(**inputs):
    x = np.asarray(inputs["x"])
    B, SL, _ = x.shape
    if SL not in _CACHE:
        _CACHE[SL] = build_program(SL)
    nc = _CACHE[SL]
    maps = make_in_maps(inputs, B, SL)
    res = run_bass_kernel_spmd(nc, maps, core_ids=list(range(B)))
    return np.stack([np.asarray(r["out"]) for r in res.results], axis=0).astype(np.float32)
```

```python
import math
import numpy as np
import concourse.bass as bass
import concourse.mybir as mybir
from concourse.bass_utils import run_bass_kernel_spmd

F32 = mybir.dt.float32
BF16 = mybir.dt.bfloat16
I32 = mybir.dt.int32
AF = mybir.ActivationFunctionType
ALU = mybir.AluOpType
AX = mybir.AxisListType

D = 1024
KD = 8
FF = 2816
EVEN_IN = 3584
ODD_IN = 6144
EPS = 1e-6
TWO_PI = 2.0 * math.pi
CW1 = 6.28125
CW2 = TWO_PI - CW1


class Res:
    __slots__ = ("w", "r")

    def __init__(self):
        self.w = None
        self.r = {}


class Sched:
    def __init__(self, nc, n_dma_sems=24):
        self.nc = nc
        self.E = ["pe", "act", "dve", "pool", "sp"]
        self.sem = {}
        self.val = {}
        self.seen = {e: {} for e in self.E}
        self._cms = []
        for e in ["pe", "act", "dve", "pool"]:
            self.sem[e] = self._alloc("sem_" + e)
            self.val[e] = 0
        self.ring = {"sp": [], "pool": []}
        for q, n in (("sp", 8), ("pool", n_dma_sems - 8)):
            for i in range(n):
                k = "dma_%s%d" % (q, i)
                self.sem[k] = self._alloc("sem_" + k)
                self.val[k] = 0
                self.ring[q].append(k)
        self.ring_i = {"sp": 0, "pool": 0}
        self.prog = {e: [] for e in self.E}

    def _alloc(self, name):
        cm = self.nc.semaphore(name)
        h = cm.__enter__()
        self._cms.append(cm)
        return h

    def _wait(self, e, ev):
        key, val = ev
        if self.seen[e].get(key, 0) >= val:
            return
        if key == e and (e == "pe" or val > self.val[e]):
            return
        sem = self.sem[key]
        self.prog[e].append(lambda eng: eng.wait_ge(sem, val))
        self.seen[e][key] = val

    def _deps(self, e, reads, writes):
        for r in reads:
            if r.w is not None:
                self._wait(e, r.w)
        for w in writes:
            if w.w is not None:
                self._wait(e, w.w)
            for k, v in w.r.items():
                self._wait(e, (k, v))

    def _commit(self, ev, reads, writes):
        for r in reads:
            if r.r.get(ev[0], 0) < ev[1]:
                r.r[ev[0]] = ev[1]
        for w in writes:
            w.w = ev
            w.r = {}

    def op(self, e, fn, reads=(), writes=(), inc=True):
        self._deps(e, reads, writes)
        if inc:
            self.val[e] += 1
            sem = self.sem[e]
            self.prog[e].append(lambda eng: fn(eng).then_inc(sem, 1))
            ev = (e, self.val[e])
        else:
            self.prog[e].append(fn)
            ev = (e, self.val[e] + 1)
        self._commit(ev, reads, writes)

    def dma(self, q, out, in_, reads=(), writes=()):
        k = self.ring[q][self.ring_i[q]]
        self.ring_i[q] = (self.ring_i[q] + 1) % len(self.ring[q])
        if self.val[k]:
            self._wait(q, (k, self.val[k]))
        self._deps(q, reads, writes)
        sem = self.sem[k]
        self.prog[q].append(lambda eng: eng.dma_start(out=out, in_=in_).then_inc(sem, 16))
        self.val[k] += 16
        ev = (k, self.val[k])
        self._commit(ev, reads, writes)

    def fence(self, res_list):
        for e in self.E:
            for r in res_list:
                if r.w is not None:
                    self._wait(e, r.w)

    def barrier(self):
        for e in self.E:
            for k in ["pe", "act", "dve", "pool"]:
                if k != e and self.val[k]:
                    self._wait(e, (k, self.val[k]))

    def finish(self, q="sp"):
        for k in self.ring["sp"] + self.ring["pool"]:
            if self.val[k]:
                self._wait(q, (k, self.val[k]))
        prog = self.prog
        with self.nc.Block() as block:
            @block.sync
            def _(eng):
                for t in prog["sp"]:
                    t(eng)

            @block.tensor
            def _(eng):
                for t in prog["pe"]:
                    t(eng)

            @block.scalar
            def _(eng):
                for t in prog["act"]:
                    t(eng)

            @block.vector
            def _(eng):
                for t in prog["dve"]:
                    t(eng)

            @block.gpsimd
            def _(eng):
                for t in prog["pool"]:
                    t(eng)


class Arena:
    def __init__(self, nc, name, nelem, dt):
        self.t = nc.sbuf_tensor(name, [128, nelem], dt).__enter__()
        self.n = nelem
        self.off = 0
        self.top = nelem

    def reset(self):
        self.off = 0

    def alloc(self, n, parts=128):
        o = self.off
        n2 = (n + 15) // 16 * 16
        self.off += n2
        assert self.off <= self.top, (self.off, self.top)
        return self.t[0:parts, o:o + n]

    def alloc_top(self, n, parts=128):
        n2 = (n + 15) // 16 * 16
        self.top -= n2
        assert self.off <= self.top, (self.off, self.top)
        return self.t[0:parts, self.top:self.top + n]


def ffn_groups():
    return [[0, 1, 2], [3, 4, 5], [6, 7, 8], [9, 10]]


def weight_schedule(dr):
    L = []

    def in_tile(tag, w, pieces):
        wv = w.rearrange("(k p) c -> p k c", p=128)
        ps = []
        o = 0
        for (c0, n) in pieces:
            ps.append((o, o + n, wv[:, :, c0:c0 + n]))
            o += n
        L.append((tag, "in", ps))

    def out_tile(tag, w, r0):
        wv = w[r0:r0 + 256, :].rearrange("(r p) c -> p r c", p=128)
        L.append((tag, "out", [(0, 1024, wv)]))

    ew, eo, ow, oo = dr["even_w_in"], dr["even_w_out"], dr["odd_w_in"], dr["odd_w_out"]
    for cp in range(2):
        for i, nm in enumerate("abc"):
            in_tile((nm, cp), ew, [(i * 512 + cp * 256, 256)])
    out_tile(("eo", 0), eo, 0)
    out_tile(("eo", 1), eo, 256)
    for hh in range(4):
        in_tile(("qf", hh), ew, [(1536 + hh * 128, 128), (2048 + hh * 128, 128)])
    for i in range(2):
        in_tile(("hi", i), ew, [(2560 + i * 256, 256)])
    for i in range(2):
        in_tile(("hg", i), ew, [(3072 + i * 256, 256)])
    out_tile(("eo", 2), eo, 512)
    out_tile(("eo", 3), eo, 768)

    def ffn(l):
        wi, wo = dr["ffn_w_in"][l], dr["ffn_w_out"][l]
        for grp in ffn_groups():
            for jp in grp:
                in_tile(("ffg", l, jp), wi, [(jp * 256, 256)])
                in_tile(("ffu", l, jp), wi, [(FF + jp * 256, 256)])
            for jp in grp:
                out_tile(("ffo", l, jp), wo, jp * 256)

    ffn(0)
    for hh in range(4):
        in_tile(("rq", hh), ow, [(hh * 256, 256)])
        in_tile(("rk", hh), ow, [(1024 + hh * 256, 256)])
        in_tile(("rv", hh, 0), ow, [(2048 + hh * 512, 256)])
        in_tile(("rv", hh, 1), ow, [(2048 + hh * 512 + 256, 256)])
        in_tile(("rg", hh, 0), ow, [(4096 + hh * 512, 256)])
        in_tile(("rg", hh, 1), ow, [(4096 + hh * 512 + 256, 256)])
        out_tile(("ro", hh, 0), oo, hh * 512)
        out_tile(("ro", hh, 1), oo, hh * 512 + 256)
    ffn(1)
    return L


class WStream:
    def __init__(self, S, nc, lst, NS):
        self.S = S
        self.NS = NS
        self.lst = lst
        self.ring = nc.sbuf_tensor("wring", [128, NS, 2048], BF16).__enter__()
        self.res = [Res() for _ in range(NS)]
        self.issued = 0
        self.consumed = 0
        self.released = [False] * len(lst)

    def view(self, i):
        s = i % self.NS
        if self.lst[i][1] == "in":
            return self.ring[:, s, :].rearrange("p (k c) -> p k c", k=8)
        return self.ring[:, s, :].rearrange("p (r c) -> p r c", r=2)

    def pump(self):
        while self.issued < len(self.lst):
            i = self.issued
            if i >= self.NS and not self.released[i - self.NS]:
                break
            v = self.view(i)
            for (c0, c1, src) in self.lst[i][2]:
                self.S.dma("pool", v[:, :, c0:c1], src, writes=[self.res[i % self.NS]])
            self.issued += 1

    def next(self, tag):
        i = self.consumed
        assert self.lst[i][0] == tag, (self.lst[i][0], tag)
        self.pump()
        assert self.issued > i, ("weight slot not available", tag)
        self.consumed += 1
        return i, self.view(i), self.res[i % self.NS]

    def done(self, i):
        self.released[i] = True
        self.pump()


def build_program(SL, NS=8):
    T = SL // 128
    GS = min(512, SL)
    NG = SL // GS
    TPG = GS // 128
    NCH = SL // 64
    CPG = GS // 64
    nc = bass.Bass("TRN2", target_bir_lowering=False)
    dr = {}

    def din(name, shape, dt=F32):
        dr[name] = nc.dram_tensor(name, shape, dt, kind="ExternalInput").ap()

    din("x", [SL, D])
    din("positions", [1, SL], I32)
    din("norm_mix_w", [128, 2 * KD])
    din("norm_ffn_w", [128, 2 * KD])
    din("norm_final_w", [1, D])
    din("even_w_in", [D, EVEN_IN])
    din("conv_w", [128, 12])
    din("hgrn_lb_logits", [128, 8])
    din("hgrn_norm_w", [1, 512])
    din("even_w_out", [1024, D])
    din("odd_w_in", [D, ODD_IN])
    din("ret_norm_w", [1, 2048])
    din("odd_w_out", [2048, D])
    din("ffn_w_in", [2, D, 2 * FF])
    din("ffn_w_out", [2, FF, D])
    din("c_ident", [128, 128])
    din("c_mask", [128, 128])
    din("c_smask", [128, 512])
    din("c_invf", [128, 1])
    din("c_rmask", [128, 512])
    din("c_rvec", [128, 12])
    out_d = nc.dram_tensor("out", [SL, D], F32, kind="ExternalOutput").ap()
    rot_d = nc.dram_tensor("rot_scratch", [2, 128, SL], F32).ap()
    r_rot = [Res(), Res()]

    S = Sched(nc)

    def sbt(name, shape, dt):
        return nc.sbuf_tensor(name, shape, dt).__enter__()

    h = sbt("h", [128, T, D], F32)
    r_h = [Res() for _ in range(T)]
    hnT = sbt("hnT", [128, KD, SL], BF16)
    r_hn = [Res() for _ in range(T)]
    W = WStream(S, nc, weight_schedule(dr), NS)
    ident_f = sbt("ident_f", [128, 128], F32)
    ident = sbt("ident", [128, 128], BF16)
    r_id = Res()
    cmask = sbt("cmask", [128, 128], F32)
    r_cm = Res()
    smask = sbt("smask", [128, 512], F32)
    r_sm = Res()
    nmw = sbt("nmw", [128, 2, KD], F32)
    nfw = sbt("nfw", [128, 2, KD], F32)
    r_nw = Res()
    small = sbt("small", [128, 256], F32)
    r_small = Res()
    AR = Arena(nc, "arena", 17600, F32)

    class _FA:
        @staticmethod
        def alloc(n, parts=128):
            return AR.alloc(n, parts)

    class _BA:
        @staticmethod
        def alloc(n, parts=128):
            m = (n + 1) // 2
            return AR.alloc(m, parts).bitcast(BF16)[:, 0:n]

    FA = _FA
    BA = _BA

    pm = [nc.psum_tensor("pm%d" % i, [128, 512], F32).__enter__() for i in range(8)]
    r_pm = [Res() for _ in range(8)]
    ptr = [pm[6 + i][:, :].bitcast(BF16) for i in range(2)]
    r_ptr = [r_pm[6], r_pm[7]]
    cnt = {"pm": 0, "ptr": 0}

    def bank():
        i = cnt["pm"] % 6
        cnt["pm"] += 1
        return pm[i], r_pm[i]

    def tbank():
        i = cnt["ptr"] % 2
        cnt["ptr"] += 1
        return ptr[i], r_ptr[i]

    def MM(out, lhsT, rhs, start, stop, reads, writes, inc):
        S.op("pe", lambda e: e.matmul(out, lhsT=lhsT, rhs=rhs, start=start, stop=stop), reads, writes, inc)

    def PROJ(out, lhs_list, rhs_list, reads, writes):
        n = len(lhs_list)
        for i in range(n):
            MM(out, lhs_list[i], rhs_list[i], i == 0, i == n - 1, reads, writes, i == n - 1)

    def TR(out, in_, np_, reads, writes, inc=True):
        idn = ident[0:np_, 0:np_]
        S.op("pe", lambda e: e.transpose(out=out, in_=in_, identity=idn), list(reads) + [r_id], writes, inc)

    def ACT(out, in_, func, reads, writes, scale=1.0, accum_out=None, bias=None):
        if bias is not None:
            S.op("act", lambda e: e.activation(out=out, in_=in_, func=func, scale=scale, bias=bias), reads, writes)
        elif accum_out is None:
            S.op("act", lambda e: e.activation(out=out, in_=in_, func=func, scale=scale), reads, writes)
        else:
            S.op("act", lambda e: e.activation(out=out, in_=in_, func=func, scale=scale, accum_out=accum_out),
                 reads, writes)

    def TT(eng, out, in0, in1, op, reads, writes):
        S.op(eng, lambda e: e.tensor_tensor(out=out, in0=in0, in1=in1, op=op), reads, writes)

    def TS(eng, out, in0, s1, s2, op0, op1, reads, writes):
        if s2 is None:
            S.op(eng, lambda e: e.tensor_scalar(out=out, in0=in0, scalar1=s1, scalar2=None, op0=op0), reads, writes)
        else:
            S.op(eng, lambda e: e.tensor_scalar(out=out, in0=in0, scalar1=s1, scalar2=s2, op0=op0, op1=op1),
                 reads, writes)

    def STT(out, in0, scalar, in1, op0, op1, reads, writes):
        S.op("dve", lambda e: e.scalar_tensor_tensor(out=out, in0=in0, scalar=scalar, in1=in1, op0=op0, op1=op1),
             reads, writes)

    def CP(eng, out, in_, reads, writes):
        S.op(eng, lambda e: e.tensor_copy(out=out, in_=in_), reads, writes)

    def RECIP(out, in_, reads, writes):
        S.op("dve", lambda e: e.reciprocal(out=out, in_=in_), reads, writes)

    def phase():
        S.barrier()
        AR.reset()

    S.dma("sp", ident_f[:], dr["c_ident"], writes=[r_id])
    S.dma("sp", cmask[:], dr["c_mask"], writes=[r_cm])
    S.dma("sp", smask[:], dr["c_smask"], writes=[r_sm])
    S.dma("sp", nmw[:].rearrange("p l k -> p (l k)"), dr["norm_mix_w"], writes=[r_nw])
    S.dma("sp", nfw[:].rearrange("p l k -> p (l k)"), dr["norm_ffn_w"], writes=[r_nw])
    S.dma("sp", small[:, 0:12], dr["conv_w"], writes=[r_small])
    S.dma("sp", small[:, 16:24], dr["hgrn_lb_logits"], writes=[r_small])
    S.dma("sp", small[:, 32:33], dr["c_invf"], writes=[r_small])
    xv = dr["x"].rearrange("(t p) d -> p t d", p=128)
    for t in range(T):
        S.dma("sp", h[:, t, :], xv[:, t, :], writes=[r_h[t]])
    CP("dve", ident[:], ident_f[:], [r_id], [r_id])
    TT("dve", small[:, 36:40], small[:, 16:20], small[:, 20:24], ALU.subtract, [r_small], [r_small])
    ACT(small[:, 40:44], small[:, 36:40], AF.Sigmoid, [r_small], [r_small])
    ACT(small[:, 44:48], small[:, 36:40], AF.Sigmoid, [r_small], [r_small], scale=-1.0)
    TS("dve", small[:, 48:52], small[:, 44:48], -1.0, None, ALU.mult, None, [r_small], [r_small])
    LB, OML, NOML = 40, 44, 48
    S.op("dve", lambda e: e.memset(small[:, 60:61], EPS), [r_small], [r_small])
    eps_col = small[:, 60:61]

    junk = sbt("junk", [128, D], BF16)
    r_junk = Res()
    xn = [sbt("xn%d" % i, [128, D], BF16) for i in range(2)]
    r_xn = [Res(), Res()]
    nst = sbt("nst", [128, 64], F32)
    r_nst = [Res() for _ in range(T)]

    def norm_gen(t, wcol, final=None):
        rn = r_nst[t]
        ACT(junk[:], h[:, t, :], AF.Square, [r_h[t]], [r_junk, rn], accum_out=nst[:, t:t + 1])
        yield
        TS("dve", nst[:, 16 + t:17 + t], nst[:, t:t + 1], 1.0 / D, EPS, ALU.mult, ALU.add, [rn], [rn])
        yield
        ACT(nst[:, 16 + t:17 + t], nst[:, 16 + t:17 + t], AF.Sqrt, [rn], [rn])
        yield
        RECIP(nst[:, 32 + t:33 + t], nst[:, 16 + t:17 + t], [rn], [rn])
        if final is not None:
            o_, ro = final["ot"][t % 2], final["r_ot"][t % 2]
            STT(o_, h[:, t, :], nst[:, 32 + t:33 + t], final["fw"], ALU.mult, ALU.mult,
                [r_h[t], rn, final["r_fw"]], [ro])
            yield
            S.dma("sp", final["ov"][:, t, :], o_, reads=[ro])
            return
        yield
        x_, rx = xn[t % 2], r_xn[t % 2]
        ACT(x_[:], h[:, t, :], AF.Copy, [r_h[t], rn], [rx], scale=nst[:, 32 + t:33 + t])
        yield
        wb = wcol.unsqueeze(2).to_broadcast([128, KD, 128])
        pt, rpt = tbank()
        for k in range(KD):
            TR(pt[:, k * 128:(k + 1) * 128], x_[:, k * 128:(k + 1) * 128], 128, [rx], [rpt], inc=(k == KD - 1))
        yield
        TT("dve", hnT[:, :, t * 128:(t + 1) * 128], pt[:].rearrange("p (k c) -> p k c", k=KD), wb, ALU.mult,
           [rpt, r_nw], [r_hn[t]])

    class NormPipe:
        def __init__(self, wcol, final=None):
            self.wcol = wcol
            self.final = final
            self.q = []
            self.fresh = []

        def push(self, t):
            g_ = norm_gen(t, self.wcol, self.final)
            next(g_)
            self.fresh.append(g_)

        def tick(self):
            for g_ in list(self.q):
                try:
                    next(g_)
                except StopIteration:
                    self.q.remove(g_)
            self.q += self.fresh
            self.fresh = []

        def flush(self):
            while self.q or self.fresh:
                self.tick()

    def hn_reads(g):
        return r_hn[g * TPG:(g + 1) * TPG]

    def out_proj_add(lhs_fn, nk, rhs_fn, reads, after_tile=None):
        for t in range(T):
            for half in range(2):
                p, rp = bank()
                PROJ(p[:, :], [lhs_fn(j, t) for j in range(nk)], [rhs_fn(j, half) for j in range(nk)], reads(t), [rp])
                hs = h[:, t, half * 512:(half + 1) * 512]
                TT("dve", hs, p[:, :], hs, ALU.add, [rp, r_h[t]], [r_h[t]])
            if after_tile is not None:
                after_tile.tick()
                after_tile.push(t)
        if after_tile is not None:
            after_tile.flush()

    np0 = NormPipe(nmw[:, 0, :])
    for t in range(T):
        np0.tick()
        np0.push(t)
    np0.flush()
    phase()
    TC = min(512, SL)
    g_cos = AR.alloc_top(TC)
    g_sin = AR.alloc_top(TC)
    tA = AR.alloc_top(TC)
    tB = AR.alloc_top(TC)
    r_tab = Res()
    RT = [r_tab]
    pos_i = tB.bitcast(I32)
    rot_stores = []

    def table_piece(ci):
        cl = slice(ci * TC, (ci + 1) * TC)
        S.dma("sp", pos_i, dr["positions"][:, cl].partition_broadcast(128), writes=RT)
        CP("dve", tA, pos_i, RT, RT)
        TS("dve", tA, tA, small[:, 32:33], None, ALU.mult, None, RT + [r_small], RT)
        TS("dve", pos_i, tA, 1.0 / TWO_PI, None, ALU.mult, None, RT, RT)
        CP("dve", g_cos, pos_i, RT, RT)
        STT(tA, g_cos, -CW1, tA, ALU.mult, ALU.add, RT, RT)
        STT(tA, g_cos, -CW2, tA, ALU.mult, ALU.add, RT, RT)

        def wrap(buf):
            TS("dve", tB, buf, math.pi, -TWO_PI, ALU.is_gt, ALU.mult, RT, RT)
            TT("dve", buf, buf, tB, ALU.add, RT, RT)
            TS("dve", tB, buf, -math.pi, TWO_PI, ALU.is_lt, ALU.mult, RT, RT)
            TT("dve", buf, buf, tB, ALU.add, RT, RT)

        wrap(tA)
        ACT(g_sin, tA, AF.Sin, RT, RT)
        TS("dve", tA, tA, math.pi / 2, None, ALU.add, None, RT, RT)
        wrap(tA)
        ACT(g_cos, tA, AF.Sin, RT, RT)
        r0, r1 = Res(), Res()
        S.dma("sp", rot_d[0][:, cl], g_cos, reads=RT, writes=[r0])
        S.dma("sp", rot_d[1][:, cl], g_sin, reads=RT, writes=[r1])
        r_tab.r[r0.w[0]] = r0.w[1]
        r_tab.r[r1.w[0]] = r1.w[1]
        rot_stores.extend([r0, r1])

    n_pieces = SL // TC
    mixT = BA.alloc(4 * SL).rearrange("p (c s) -> p c s", c=4)
    r_mix = [Res() for _ in range(NG)]
    ub = [FA.alloc(SL + 2), FA.alloc(SL + 2)]
    r_ub = [Res(), Res()]
    a_sb = [FA.alloc(GS), FA.alloc(GS)]
    r_asb = [Res(), Res()]
    tcv = [FA.alloc(GS), FA.alloc(GS)]
    r_tcv = [Res(), Res()]
    for i in range(2):
        S.op("dve", lambda e, i=i: e.memset(ub[i][:, 0:2], 0.0), [], [r_ub[i]])
    it = 0
    for cp in range(2):
        ia, wa, ra = W.next(("a", cp))
        ib, wb_, rb = W.next(("b", cp))
        ic, wc, rc = W.next(("c", cp))
        for ci in range(2):
            cc = cp * 2 + ci
            u, ru = ub[cc % 2], r_ub[cc % 2]
            cs = slice(ci * 128, (ci + 1) * 128)
            for g in range(NG):
                gsl = slice(g * GS, (g + 1) * GS)
                rhs = [hnT[:, k, gsl] for k in range(KD)]
                pa, rpa = bank()
                PROJ(pa[:, 0:GS], [wa[:, k, cs] for k in range(KD)], rhs, [ra] + hn_reads(g), [rpa])
                pc, rpc = bank()
                PROJ(pc[:, 0:GS], [wc[:, k, cs] for k in range(KD)], rhs, [rc] + hn_reads(g), [rpc])
                pb, rpb = bank()
                PROJ(pb[:, 0:GS], [wb_[:, k, cs] for k in range(KD)], rhs, [rb] + hn_reads(g), [rpb])
                asb, rasb = a_sb[it % 2], r_asb[it % 2]
                tc_, rtc = tcv[it % 2], r_tcv[it % 2]
                it += 1
                ACT(asb[:, :], pa[:, 0:GS], AF.Copy, [rpa], [rasb])
                TT("dve", u[:, 2 + g * GS:2 + (g + 1) * GS], pc[:, 0:GS], asb[:, :], ALU.mult, [rpc, rasb], [ru])
                TS("dve", tc_[:, :], u[:, 2 + g * GS:2 + (g + 1) * GS], small[:, 8 + cc:9 + cc], None, ALU.mult, None,
                   [ru, r_small], [rtc])
                STT(tc_[:, :], u[:, 1 + g * GS:1 + (g + 1) * GS], small[:, 4 + cc:5 + cc], tc_[:, :], ALU.mult, ALU.add,
                    [ru, rtc, r_small], [rtc])
                STT(tc_[:, :], u[:, g * GS:(g + 1) * GS], small[:, cc:cc + 1], tc_[:, :], ALU.mult, ALU.add,
                    [ru, rtc, r_small], [rtc])
                TT("dve", mixT[:, cc, gsl], pb[:, 0:GS], tc_[:, :], ALU.mult, [rpb, rtc], [r_mix[g]])
            if cc < n_pieces:
                table_piece(cc)
        W.done(ia)
        W.done(ib)
        W.done(ic)
    i0, wo0, ro0 = W.next(("eo", 0))
    i1, wo1, ro1 = W.next(("eo", 1))
    wos = [wo0, wo1]
    out_proj_add(lambda j, t: mixT[:, j, t * 128:(t + 1) * 128], 4,
                 lambda j, half: wos[j // 2][:, j % 2, half * 512:(half + 1) * 512],
                 lambda t: [r_mix[t // TPG], ro0, ro1])
    W.done(i0)
    W.done(i1)

    for ci in range(4, n_pieces):
        table_piece(ci)
    S.fence(rot_stores)
    AR.top = AR.n
    phase()
    gwn = FA.alloc(512, parts=64)
    r_gwn = Res()
    S.dma("sp", gwn, dr["hgrn_norm_w"].partition_broadcast(64), writes=[r_gwn])
    qt = BA.alloc(4 * SL).rearrange("p (h s) -> p h s", h=4)
    kt = BA.alloc(4 * SL).rearrange("p (h s) -> p h s", h=4)
    r_qk = [[Res() for _ in range(NG)] for _ in range(4)]
    Eh = FA.alloc(4 * NCH).rearrange("p (h c) -> p h c", h=4)
    r_E = Res()
    markA = AR.off
    tmpA = [{n: FA.alloc(GS) for n in ["sg", "qs", "kk", "bb", "eb", "enb", "sqt"]} for _ in range(2)]
    r_tmpA = [{n: Res() for n in ["sg", "qs", "kk", "bb", "eb", "enb", "sqt"]} for _ in range(2)]
    qf_tiles = {}

    def stageA(it, hh, g):
        tm, rt = tmpA[it % 2], r_tmpA[it % 2]
        sg, qs, kk, bb, eb, enb, sqt = (tm[n] for n in ["sg", "qs", "kk", "bb", "eb", "enb", "sqt"])
        if g == 0:
            qf_tiles[hh] = W.next(("qf", hh))
        iqf, wqf, rqf = qf_tiles[hh]
        lbc = small[:, LB + hh:LB + hh + 1]
        omlc = small[:, OML + hh:OML + hh + 1]
        nomlc = small[:, NOML + hh:NOML + hh + 1]
        gsl = slice(g * GS, (g + 1) * GS)
        rhs = [hnT[:, k, gsl] for k in range(KD)]
        pq, rpq = bank()
        PROJ(pq[:, 0:GS], [wqf[:, k, 0:128] for k in range(KD)], rhs, [rqf] + hn_reads(g), [rpq])
        pf, rpf = bank()
        PROJ(pf[:, 0:GS], [wqf[:, k, 128:256] for k in range(KD)], rhs, [rqf] + hn_reads(g), [rpf])
        if g == NG - 1:
            W.done(iqf)
        ACT(sg[:, :], pf[:, 0:GS], AF.Sigmoid, [rpf], [rt["sg"]])
        ACT(sqt[:, :], pq[:, 0:GS], AF.Sigmoid, [rpq], [rt["sqt"]])
        TT("dve", qs[:, :], pq[:, 0:GS], sqt[:, :], ALU.mult, [rpq, rt["sqt"]], [rt["qs"]])
        TS("dve", kk[:, :], sg[:, :], nomlc, omlc, ALU.mult, ALU.add, [rt["sg"], r_small], [rt["kk"]])
        TS("dve", sg[:, :], sg[:, :], omlc, lbc, ALU.mult, ALU.add, [rt["sg"], r_small], [rt["sg"]])
        yield
        S.op("dve", lambda e: e.tensor_tensor_scan(out=eb[:, :], data0=smask[:, 0:GS], data1=sg[:, :], initial=0.0,
                                                   op0=ALU.max, op1=ALU.mult),
             [rt["sg"], r_sm], [rt["eb"]])
        RECIP(enb[:, :], eb[:, :], [rt["eb"]], [rt["enb"]])
        TT("pool", qt[:, hh, gsl], qs[:, :], eb[:, :], ALU.mult, [rt["qs"], rt["eb"]], [r_qk[hh][g]])
        TT("pool", kt[:, hh, gsl], kk[:, :], enb[:, :], ALU.mult, [rt["kk"], rt["enb"]], [r_qk[hh][g]])
        CP("dve", Eh[:, hh, g * CPG:(g + 1) * CPG], eb[:, :].rearrange("p (c j) -> p c j", j=64)[:, :, 63],
           [rt["eb"]], [r_E])
        yield

    gensA = [stageA(i, i // NG, i % NG) for i in range(4 * NG)]
    for i in range(4 * NG + 1):
        if i < 4 * NG:
            next(gensA[i])
        if i >= 1:
            next(gensA[i - 1])
    S.barrier()
    AR.off = markA

    whi = [W.next(("hi", i)) for i in range(2)]
    whg = [W.next(("hg", i)) for i in range(2)]
    weo = [W.next(("eo", 2 + i)) for i in range(2)]
    V_c = [BA.alloc(512, parts=64) for _ in range(2)]
    r_Vc = [Res(), Res()]
    gs_c = FA.alloc(512, parts=64)
    r_gsc = Res()
    GW_t = [BA.alloc(1024, parts=64).rearrange("p (c v) -> p c v", c=2) for _ in range(3)]
    r_GW = [[Res(), Res()] for _ in range(3)]
    o_t = [FA.alloc(1024, parts=64).rearrange("p (c v) -> p c v", c=2) for _ in range(2)]
    r_ot = [[Res(), Res()], [Res(), Res()]]
    ktc = [BA.alloc(512, parts=64) for _ in range(2)]
    r_ktc = [Res(), Res()]
    sTc = [BA.alloc(256, parts=64) for _ in range(2)]
    r_sTc = [Res(), Res()]
    U4 = FA.alloc(512).rearrange("p (h v) -> p h v", h=4)
    r_U4 = [Res() for _ in range(4)]
    st4 = [BA.alloc(512).rearrange("p (h v) -> p h v", h=4) for _ in range(2)]
    r_st4 = [[Res() for _ in range(4)] for _ in range(2)]
    hstt = [FA.alloc(32, parts=64) for _ in range(2)]
    r_hstt = [Res(), Res()]
    mixt = [BA.alloc(512).rearrange("p (h s) -> p h s", h=4) for _ in range(2)]
    r_mixt = [Res(), Res()]
    pkT = pm[6][0:64, :].bitcast(BF16)
    pyT = pm[7][:, :].bitcast(BF16)
    mask4 = cmask[0:64, 0:64].unsqueeze(1).to_broadcast([64, 4, 64])

    def chunk_gen(c):
        csl = slice(c * 64, (c + 1) * 64)
        g = c // CPG
        t = c // 2
        cj = c % 2
        lhs = [hnT[:, k, csl] for k in range(KD)]
        for i in range(2):
            PROJ(pm[0][0:64, i * 256:(i + 1) * 256], lhs, [whi[i][1][:, k, :] for k in range(KD)],
                 [whi[i][2], r_hn[t]], [r_pm[0]])
        Vc, rVc = V_c[c % 2], r_Vc[c % 2]
        ACT(Vc, pm[0][0:64, :], AF.Copy, [r_pm[0]], [rVc])
        for i in range(2):
            PROJ(pm[1][0:64, i * 256:(i + 1) * 256], lhs, [whg[i][1][:, k, :] for k in range(KD)],
                 [whg[i][2], r_hn[t]], [r_pm[1]])
        ACT(gs_c, pm[1][0:64, :], AF.Silu, [r_pm[1]], [r_gsc])
        GW, rGW = GW_t[t % 3], r_GW[t % 3]
        TT("pool", GW[:, cj, :], gs_c, gwn, ALU.mult, [r_gsc, r_gwn], [rGW[cj]])
        for hh in range(4):
            TR(pkT[:, hh * 128:(hh + 1) * 128], kt[:, hh, csl], 128, [r_qk[hh][g]], [r_pm[6]], inc=(hh == 3))
        kc, rkc = ktc[c % 2], r_ktc[c % 2]
        CP("dve", kc, pkT[:, 0:512], [r_pm[6]], [rkc])
        for hh in range(4):
            MM(pm[2][0:64, hh * 64:(hh + 1) * 64], kt[:, hh, csl], qt[:, hh, csl], True, True, [r_qk[hh][g]], [r_pm[2]],
               hh == 3)
        sT, rsT = sTc[c % 2], r_sTc[c % 2]
        TT("dve", sT.rearrange("p (h i) -> p h i", h=4), pm[2][0:64, 0:256].rearrange("p (h i) -> p h i", h=4), mask4,
           ALU.mult, [r_pm[2], r_cm], [rsT])
        yield
        for hh in range(4):
            hs_ = slice(hh * 128, (hh + 1) * 128)
            MM(pm[3][0:64, hs_], sT[:, hh * 64:(hh + 1) * 64], Vc[:, hs_], True, c == 0, [rsT, rVc], [r_pm[3]],
               c == 0 and hh == 3)
            if c > 0:
                MM(pm[3][0:64, hs_], qt[:, hh, csl], st4[(c - 1) % 2][:, hh, :], False, True,
                   [r_qk[hh][g], r_st4[(c - 1) % 2][hh]], [r_pm[3]], hh == 3)
        ot_, rot_ = o_t[t % 2], r_ot[t % 2]
        ACT(ot_[:, cj, :], pm[3][0:64, :], AF.Copy, [r_pm[3]], [rot_[cj]])
        if c < NCH - 1:
            for hh in range(4):
                hs_ = slice(hh * 128, (hh + 1) * 128)
                MM(pm[4][:, hs_], kc[:, hs_], Vc[:, hs_], True, True, [rkc, rVc], [r_pm[4]], hh == 3)
            for hh in range(4):
                hs_ = slice(hh * 128, (hh + 1) * 128)
                if c == 0:
                    CP("dve", U4[:, hh, :], pm[4][:, hs_], [r_pm[4]], [r_U4[hh]])
                else:
                    STT(U4[:, hh, :], U4[:, hh, :], Eh[:, hh, c - 1:c], pm[4][:, hs_], ALU.mult, ALU.add,
                        [r_U4[hh], r_E, r_pm[4]], [r_U4[hh]])
                TS("dve", st4[c % 2][:, hh, :], U4[:, hh, :], Eh[:, hh, c:c + 1], None, ALU.mult, None,
                   [r_U4[hh], r_E], [r_st4[c % 2][hh]])
        yield

    def tile_gen(t):
        ot_, rot_ = o_t[t % 2], r_ot[t % 2]
        GW, rGW = GW_t[t % 3], r_GW[t % 3]
        hs, rhs_ = hstt[t % 2], r_hstt[t % 2]
        for cj in range(2):
            for hh in range(4):
                ACT(junk[0:64, 0:128], ot_[:, cj, hh * 128:(hh + 1) * 128], AF.Square, [rot_[cj]], [r_junk, rhs_],
                    accum_out=hs[:, cj * 4 + hh:cj * 4 + hh + 1])
        TS("dve", hs[:, 8:16], hs[:, 0:8], 1.0 / 128, EPS, ALU.mult, ALU.add, [rhs_], [rhs_])
        ACT(hs[:, 8:16], hs[:, 8:16], AF.Sqrt, [rhs_], [rhs_])
        RECIP(hs[:, 16:24], hs[:, 8:16], [rhs_], [rhs_])
        yield
        o4 = ot_[:, :, :].rearrange("p c (h v) -> p (c h) v", h=4)
        rstd_b = hs[:, 16:24].unsqueeze(2).to_broadcast([64, 8, 128])
        TT("dve", o4, o4, rstd_b, ALU.mult, [rot_[0], rot_[1], rhs_], [rot_[0], rot_[1]])
        TT("pool", GW[:, :, :], ot_[:, :, :], GW[:, :, :], ALU.mult, [rot_[0], rot_[1], rGW[0], rGW[1]], [rGW[0], rGW[1]])
        yield
        for cj in range(2):
            for hh in range(4):
                TR(pyT[:, hh * 128 + cj * 64:hh * 128 + (cj + 1) * 64], GW[:, cj, hh * 128:(hh + 1) * 128], 64,
                   [rGW[cj]], [r_pm[7]], inc=(cj == 1 and hh == 3))
        mx, rmx = mixt[t % 2], r_mixt[t % 2]
        ACT(mx, pyT[:, 0:512].rearrange("p (h s) -> p h s", h=4), AF.Copy, [r_pm[7]], [rmx])
        yield
        for half in range(2):
            PROJ(pm[5][:, :], [mx[:, j, :] for j in range(4)],
                 [weo[j // 2][1][:, j % 2, half * 512:(half + 1) * 512] for j in range(4)],
                 [rmx, weo[0][2], weo[1][2]], [r_pm[5]])
            hsl = h[:, t, half * 512:(half + 1) * 512]
            TT("dve", hsl, pm[5][:, :], hsl, ALU.add, [r_pm[5], r_h[t]], [r_h[t]])
            yield

    npH = NormPipe(nfw[:, 0, :])
    cg = [chunk_gen(c) for c in range(NCH)]
    tg = [tile_gen(t) for t in range(T)]
    tg_calls = [0] * T

    def tstep(t, n):
        if 0 <= t < T:
            assert tg_calls[t] == n, (t, tg_calls[t], n)
            next(tg[t])
            tg_calls[t] += 1

    for i in range(NCH + 8):
        if i < NCH:
            next(cg[i])
        if 0 <= i - 1 < NCH:
            next(cg[i - 1])
        if i % 2 == 0:
            tstep((i - 2) // 2, 0)
            tstep((i - 4) // 2, 2)
            tstep((i - 6) // 2, 4)
        else:
            tstep((i - 3) // 2, 1)
            tstep((i - 5) // 2, 3)
    assert all(n == 5 for n in tg_calls), tg_calls
    for w_ in whi + whg + weo:
        W.done(w_[0])
    for t in range(T):
        npH.tick()
        npH.push(t)
    npH.flush()

    def ffn(l, tail_setup, tail_tile):
        phase()
        tail_setup()
        actT = [BA.alloc(6 * SL).rearrange("p (c s) -> p c s", c=6) for _ in range(2)]
        r_act = [[[Res() for _ in range(NG)] for _ in range(6)] for _ in range(2)]
        sgt = [FA.alloc(GS), FA.alloc(GS)]
        r_sgt = [Res(), Res()]
        it = 0
        for gi, grp in enumerate(ffn_groups()):
            aT, rA = actT[gi % 2], r_act[gi % 2]
            for li, jp in enumerate(grp):
                ig_, wg, rg = W.next(("ffg", l, jp))
                iu_, wu, ru = W.next(("ffu", l, jp))
                for ci in range(2):
                    jj = li * 2 + ci
                    cs = slice(ci * 128, (ci + 1) * 128)
                    for g in range(NG):
                        gsl = slice(g * GS, (g + 1) * GS)
                        rhs = [hnT[:, k, gsl] for k in range(KD)]
                        pg, rpg = bank()
                        PROJ(pg[:, 0:GS], [wg[:, k, cs] for k in range(KD)], rhs, [rg] + hn_reads(g), [rpg])
                        pu, rpu = bank()
                        PROJ(pu[:, 0:GS], [wu[:, k, cs] for k in range(KD)], rhs, [ru] + hn_reads(g), [rpu])
                        st_, rst = sgt[it % 2], r_sgt[it % 2]
                        it += 1
                        ACT(st_[:, :], pg[:, 0:GS], AF.Silu, [rpg], [rst])
                        TT("dve", aT[:, jj, gsl], pu[:, 0:GS], st_[:, :], ALU.mult, [rpu, rst], [rA[jj][g]])
                W.done(ig_)
                W.done(iu_)
            wts = [W.next(("ffo", l, jp)) for jp in grp]
            nk = 2 * len(grp)
            out_proj_add(lambda j, t: aT[:, j, t * 128:(t + 1) * 128], nk,
                         lambda j, half: wts[j // 2][1][:, j % 2, half * 512:(half + 1) * 512],
                         lambda t: [rA[j][t // TPG] for j in range(nk)] + [w_[2] for w_ in wts],
                         after_tile=(tail_tile if gi == len(ffn_groups()) - 1 else None))
            for w_ in wts:
                W.done(w_[0])

    rot_sb = {}

    def rot_prefetch():
        rot_sb["cos"] = AR.alloc_top(SL)
        rot_sb["sin"] = AR.alloc_top(SL)
        rot_sb["r"] = Res()
        S.dma("sp", rot_sb["cos"], rot_d[0], reads=rot_stores, writes=[rot_sb["r"]])
        S.dma("sp", rot_sb["sin"], rot_d[1], reads=rot_stores, writes=[rot_sb["r"]])

    ffn(0, rot_prefetch, NormPipe(nmw[:, 1, :]))

    phase()
    cosT, sinT, r_cs = rot_sb["cos"], rot_sb["sin"], rot_sb["r"]
    rmask = FA.alloc(512)
    rvec = FA.alloc(16)
    r_rc = Res()
    S.dma("sp", rmask, dr["c_rmask"], writes=[r_rc])
    S.dma("sp", rvec[:, 0:12], dr["c_rvec"], writes=[r_rc])
    rnw = FA.alloc(512)
    r_rnw = Res()
    qT = BA.alloc(2 * SL).rearrange("p (a s) -> p a s", a=2)
    kT = BA.alloc(2 * SL).rearrange("p (a s) -> p a s", a=2)
    r_q = [Res() for _ in range(NG)]
    r_k = [Res() for _ in range(NG)]
    HG = GS // 2
    tt = [[FA.alloc(HG) for _ in range(4)] for _ in range(2)]
    r_tt = [[Res() for _ in range(4)] for _ in range(2)]
    V_sb = [BA.alloc(512), BA.alloc(512)]
    r_V = [Res(), Res()]
    gsb = [FA.alloc(512), FA.alloc(512)]
    r_gs = [Res(), Res()]
    ki_sb = [BA.alloc(256), BA.alloc(256)]
    r_ki = [Res(), Res()]
    sT2 = [BA.alloc(128), BA.alloc(128)]
    r_sT2 = [Res(), Res()]
    Ur = FA.alloc(1024).rearrange("p (a v) -> p a v", a=2)
    r_Ur = Res()
    stb = [BA.alloc(1024).rearrange("p (a v) -> p a v", a=2) for _ in range(2)]
    r_stb2 = [Res(), Res()]
    bnst = [FA.alloc(16), FA.alloc(16)]
    r_bn = [Res(), Res()]
    y1 = FA.alloc(512)
    r_y1 = Res()
    y_sb = [BA.alloc(512) for _ in range(3)]
    r_ysb = [Res() for _ in range(3)]
    yT = [BA.alloc(512).rearrange("p (j c) -> p j c", j=4) for _ in range(2)]
    r_yT = [Res(), Res()]
    rot_it = [0]
    hb_ps = pm[6][:, 0:128]
    hb_pt2 = pm[6][:, 256:512].bitcast(BF16)
    hb_pt = pm[7][:, 0:128].bitcast(BF16)
    r_hb = [r_pm[6], r_pm[7], r_pm[6]]
    for hh in range(4):
        gC = (1.0 - 2.0 ** (-5 - hh)) ** 128
        S.dma("sp", rnw, dr["ret_norm_w"][:, hh * 512:(hh + 1) * 512].partition_broadcast(128), writes=[r_rnw])
        mask_h = rmask[:, hh * 128:(hh + 1) * 128]
        c_h = rvec[:, hh:hh + 1]
        c2_h = rvec[:, 4 + hh:5 + hh]
        dk_h = rvec[:, 8 + hh:9 + hh]
        for (tag, dst, rdst) in (("rq", qT, r_q), ("rk", kT, r_k)):
            iw, ww, rw = W.next((tag, hh))
            for g in range(NG):
                gsl = slice(g * GS, (g + 1) * GS)
                rhs = [hnT[:, k, gsl] for k in range(KD)]
                pA, rpA = bank()
                PROJ(pA[:, 0:GS], [ww[:, k, 0:128] for k in range(KD)], rhs, [rw] + hn_reads(g), [rpA])
                pB, rpB = bank()
                PROJ(pB[:, 0:GS], [ww[:, k, 128:256] for k in range(KD)], rhs, [rw] + hn_reads(g), [rpB])
                for hf in range(2):
                    cl = slice(hf * HG, (hf + 1) * HG)
                    al = slice(g * GS + hf * HG, g * GS + (hf + 1) * HG)
                    t_, rt_ = tt[rot_it[0] % 2], r_tt[rot_it[0] % 2]
                    rot_it[0] += 1
                    TT("dve", t_[0], pA[:, cl], cosT[:, al], ALU.mult, [rpA, r_cs], [rt_[0]])
                    TT("dve", t_[1], pB[:, cl], sinT[:, al], ALU.mult, [rpB, r_cs], [rt_[1]])
                    TT("dve", t_[2], pA[:, cl], sinT[:, al], ALU.mult, [rpA, r_cs], [rt_[2]])
                    TT("dve", t_[3], pB[:, cl], cosT[:, al], ALU.mult, [rpB, r_cs], [rt_[3]])
                    TT("pool", dst[:, 0, al], t_[0], t_[1], ALU.subtract, [rt_[0], rt_[1]], [rdst[g]])
                    TT("pool", dst[:, 1, al], t_[2], t_[3], ALU.add, [rt_[2], rt_[3]], [rdst[g]])
            W.done(iw)
        wv = [W.next(("rv", hh, i)) for i in range(2)]
        wg_ = [W.next(("rg", hh, i)) for i in range(2)]
        wo_ = [W.next(("ro", hh, i)) for i in range(2)]

        def ret_tile(t):
            tsl = slice(t * 128, (t + 1) * 128)
            g = t // TPG
            lhs = [hnT[:, k, tsl] for k in range(KD)]
            pv, rpv = pm[0], r_pm[0]
            for i in range(2):
                PROJ(pv[:, i * 256:(i + 1) * 256], lhs, [wv[i][1][:, k, :] for k in range(KD)], [wv[i][2], r_hn[t]], [rpv])
            V, rV = V_sb[t % 2], r_V[t % 2]
            ACT(V, pv[:, :], AF.Copy, [rpv], [rV])
            pg, rpg = pm[1], r_pm[1]
            for i in range(2):
                PROJ(pg[:, i * 256:(i + 1) * 256], lhs, [wg_[i][1][:, k, :] for k in range(KD)], [wg_[i][2], r_hn[t]], [rpg])
            gs_, rgs = gsb[t % 2], r_gs[t % 2]
            ACT(gs_, pg[:, :], AF.Silu, [rpg], [rgs])
            TT("pool", gs_, gs_, rnw, ALU.mult, [rgs, r_rnw], [rgs])
            pt, rpt = hb_pt, r_hb[1]
            for a in range(2):
                TR(pt[:, a * 128:(a + 1) * 128], kT[:, a, tsl], 128, [r_k[g]], [rpt], inc=(a == 1))
            ki, rki = ki_sb[t % 2], r_ki[t % 2]
            TS("dve", ki, pt[:, 0:256], dk_h, None, ALU.mult, None, [rpt, r_rc], [rki])
            ps_, rps = hb_ps, r_hb[0]
            PROJ(ps_[:, 0:128], [kT[:, a, tsl] for a in range(2)], [qT[:, a, tsl] for a in range(2)],
                 [r_k[g], r_q[g]], [rps])
            sT, rsT = sT2[t % 2], r_sT2[t % 2]
            TT("dve", sT, ps_[:, 0:128], mask_h, ALU.mult, [rps, r_rc], [rsT])
            yield
            po, rpo = pm[2], r_pm[2]
            MM(po[:, :], sT, V, True, t == 0, [rsT, rV], [rpo], t == 0)
            if t > 0:
                sb_, rsb = stb[(t - 1) % 2], r_stb2[(t - 1) % 2]
                for a in range(2):
                    MM(po[:, :], qT[:, a, tsl], sb_[:, a, :], False, a == 1, [r_q[g], rsb], [rpo], a == 1)
            pkvs = []
            if t < T - 1:
                for a in range(2):
                    pkv, rpkv = pm[3 + a], r_pm[3 + a]
                    MM(pkv[:, :], ki[:, a * 128:(a + 1) * 128], V, True, True, [rki, rV], [rpkv], True)
                    pkvs.append((pkv, rpkv))
            bn, rbn = bnst[t % 2], r_bn[t % 2]
            S.op("dve", lambda e: e.bn_stats(out=bn[:, 0:6], in_=po[:, :]), [rpo], [rbn])
            S.op("dve", lambda e: e.bn_aggr(out=bn[:, 8:10], in_=bn[:, 0:6]), [rbn], [rbn])
            TS("dve", bn[:, 10:11], bn[:, 9:10], c2_h, EPS, ALU.mult, ALU.add, [rbn, r_rc], [rbn])
            ACT(bn[:, 10:11], bn[:, 10:11], AF.Sqrt, [rbn], [rbn])
            for a, (pkv, rpkv) in enumerate(pkvs):
                if t == 0:
                    CP("dve", Ur[:, a, :], pkv[:, :], [rpkv], [r_Ur])
                else:
                    STT(Ur[:, a, :], Ur[:, a, :], gC, pkv[:, :], ALU.mult, ALU.add, [r_Ur, rpkv], [r_Ur])
            if pkvs:
                TS("pool", stb[t % 2][:, :, :], Ur[:, :, :], gC, 1.0, ALU.mult, ALU.mult, [r_Ur], [r_stb2[t % 2]])
            RECIP(bn[:, 11:12], bn[:, 10:11], [rbn], [rbn])
            TS("dve", bn[:, 11:12], bn[:, 11:12], c_h, None, ALU.mult, None, [rbn, r_rc], [rbn])
            TS("dve", y1, po[:, :], bn[:, 8:9], bn[:, 11:12], ALU.subtract, ALU.mult, [rpo, rbn], [r_y1])
            ys, rys = y_sb[t % 3], r_ysb[t % 3]
            TT("pool", ys, y1, gs_, ALU.mult, [r_y1, rgs], [rys])
            yield
            pt2, rpt2 = hb_pt2, r_hb[2]
            for j in range(4):
                TR(pt2[:, j * 128:(j + 1) * 128], ys[:, j * 128:(j + 1) * 128], 128, [rys], [rpt2], inc=(j == 3))
            yT_, ryT = yT[t % 2], r_yT[t % 2]
            ACT(yT_, pt2[:, 0:512].rearrange("p (j c) -> p j c", j=4), AF.Copy, [rpt2], [ryT])
            yield
            for half in range(2):
                p, rp = pm[5], r_pm[5]
                PROJ(p[:, :], [yT_[:, j, :] for j in range(4)],
                     [wo_[j // 2][1][:, j % 2, half * 512:(half + 1) * 512] for j in range(4)],
                     [ryT, wo_[0][2], wo_[1][2]], [rp])
                hs = h[:, t, half * 512:(half + 1) * 512]
                TT("dve", hs, p[:, :], hs, ALU.add, [rp, r_h[t]], [r_h[t]])
                yield

        npR = NormPipe(nfw[:, 1, :])
        gens = [ret_tile(t) for t in range(T)]
        stage_of = [0] * T
        for i in range(T + 3):
            for (lag, st_) in ((3, 2), (0, 0), (3, 3), (1, 1), (3, 4)):
                t = i - lag
                if 0 <= t < T:
                    assert stage_of[t] == st_, (t, stage_of[t], st_)
                    next(gens[t])
                    stage_of[t] += 1
        for w_ in wv + wg_ + wo_:
            W.done(w_[0])
    for t in range(T):
        npR.tick()
        npR.push(t)
    npR.flush()
    AR.top = AR.n

    fin = {}
    ov = out_d.rearrange("(t p) d -> p t d", p=128)

    fin["ov"] = ov

    def final_setup():
        fin["fw"] = FA.alloc(D)
        fin["r_fw"] = Res()
        S.dma("sp", fin["fw"], dr["norm_final_w"].partition_broadcast(128), writes=[fin["r_fw"]])
        fin["ot"] = [FA.alloc(D), FA.alloc(D)]
        fin["r_ot"] = [Res(), Res()]

    ffn(1, final_setup, NormPipe(None, final=fin))
    assert W.consumed == len(W.lst)
    S.finish("sp")
    return nc


def make_consts():
    c = {}
    c["c_ident"] = np.eye(128, dtype=np.float32)
    j = np.arange(128)
    c["c_mask"] = (j[:, None] <= j[None, :]).astype(np.float32)
    sm = np.zeros((128, 512), dtype=np.float32)
    sm[:, ::64] = 1.0
    c["c_smask"] = sm
    c["c_invf"] = (10000.0 ** (-np.arange(128, dtype=np.float64) / 128)).astype(np.float32).reshape(128, 1)
    rm = np.zeros((128, 4, 128))
    rv = np.zeros((128, 12))
    for hh in range(4):
        gam = 1.0 - 2.0 ** (-5 - hh)
        kinv = gam ** (-(j + 1.0)) * (256 ** -0.5)
        rm[:, hh, :] = (j[:, None] <= j[None, :]) * kinv[:, None]
        rv[:, hh] = gam ** (j + 1.0)
        rv[:, 4 + hh] = gam ** (2 * (j + 1.0))
        rv[:, 8 + hh] = kinv
    c["c_rmask"] = rm.reshape(128, 512).astype(np.float32)
    c["c_rvec"] = rv.astype(np.float32)
    return c


def make_in_maps(inputs, n, SL):
    f = lambda a: np.ascontiguousarray(np.asarray(a))
    shared = {
        "norm_mix_w": f(f(inputs["norm_mix_w"]).reshape(2, KD, 128).transpose(2, 0, 1)).reshape(128, 2 * KD),
        "norm_ffn_w": f(f(inputs["norm_ffn_w"]).reshape(2, KD, 128).transpose(2, 0, 1)).reshape(128, 2 * KD),
        "norm_final_w": f(inputs["norm_final_w"]).reshape(1, D),
        "even_w_in": f(inputs["even_w_in"])[0],
        "conv_w": f(f(inputs["conv_w"])[0].reshape(3, 4, 128).transpose(2, 0, 1)).reshape(128, 12),
        "hgrn_lb_logits": f(f(inputs["hgrn_lb_logits"]).reshape(2, 4, 128).transpose(2, 0, 1)).reshape(128, 8),
        "hgrn_norm_w": f(inputs["hgrn_norm_w"]).reshape(1, 512),
        "even_w_out": f(inputs["even_w_out"])[0],
        "odd_w_in": f(inputs["odd_w_in"])[0],
        "ret_norm_w": f(inputs["ret_norm_w"]).reshape(1, 2048),
        "odd_w_out": f(inputs["odd_w_out"])[0],
        "ffn_w_in": f(inputs["ffn_w_in"]),
        "ffn_w_out": f(inputs["ffn_w_out"]),
    }
    shared.update(make_consts())
    x = f(inputs["x"])
    pos = f(inputs["positions"]).astype(np.int32)
    maps = []
    for b in range(n):
        m = dict(shared)
        m["x"] = np.ascontiguousarray(x[b])
        m["positions"] = np.ascontiguousarray(pos[b].reshape(1, SL))
        maps.append(m)
    return maps


_CACHE = {}


def kernel(**inputs):
    x = np.asarray(inputs["x"])
    B, SL, _ = x.shape
    if SL not in _CACHE:
        _CACHE[SL] = build_program(SL)
    nc = _CACHE[SL]
    maps = make_in_maps(inputs, B, SL)
    res = run_bass_kernel_spmd(nc, maps, core_ids=list(range(B)))
    return np.stack([np.asarray(r["out"]) for r in res.results], axis=0).astype(np.float32)
```

```python
import math
import numpy as np
import concourse.bass as bass
import concourse.mybir as mybir
from concourse.bass_utils import run_bass_kernel_spmd

F32 = mybir.dt.float32
BF16 = mybir.dt.bfloat16
I32 = mybir.dt.int32
AF = mybir.ActivationFunctionType
ALU = mybir.AluOpType
AX = mybir.AxisListType

D = 1024
KD = 8
FF = 2816
EVEN_IN = 3584
ODD_IN = 6144
EPS = 1e-6
TWO_PI = 2.0 * math.pi
CW1 = 6.28125
CW2 = TWO_PI - CW1


class Res:
    __slots__ = ("w", "r")

    def __init__(self):
        self.w = None
        self.r = {}


class Sched:
    def __init__(self, nc, n_dma_sems=24):
        self.nc = nc
        self.E = ["pe", "act", "dve", "pool", "sp"]
        self.sem = {}
        self.val = {}
        self.seen = {e: {} for e in self.E}
        self._cms = []
        for e in ["pe", "act", "dve", "pool"]:
            self.sem[e] = self._alloc("sem_" + e)
            self.val[e] = 0
        self.ring = {"sp": [], "pool": []}
        for q, n in (("sp", 8), ("pool", n_dma_sems - 8)):
            for i in range(n):
                k = "dma_%s%d" % (q, i)
                self.sem[k] = self._alloc("sem_" + k)
                self.val[k] = 0
                self.ring[q].append(k)
        self.ring_i = {"sp": 0, "pool": 0}
        self.prog = {e: [] for e in self.E}

    def _alloc(self, name):
        cm = self.nc.semaphore(name)
        h = cm.__enter__()
        self._cms.append(cm)
        return h

    def _wait(self, e, ev):
        key, val = ev
        if self.seen[e].get(key, 0) >= val:
            return
        if key == e and (e == "pe" or val > self.val[e]):
            return
        sem = self.sem[key]
        self.prog[e].append(lambda eng: eng.wait_ge(sem, val))
        self.seen[e][key] = val

    def _deps(self, e, reads, writes):
        for r in reads:
            if r.w is not None:
                self._wait(e, r.w)
        for w in writes:
            if w.w is not None:
                self._wait(e, w.w)
            for k, v in w.r.items():
                self._wait(e, (k, v))

    def _commit(self, ev, reads, writes):
        for r in reads:
            if r.r.get(ev[0], 0) < ev[1]:
                r.r[ev[0]] = ev[1]
        for w in writes:
            w.w = ev
            w.r = {}

    def op(self, e, fn, reads=(), writes=(), inc=True):
        self._deps(e, reads, writes)
        if inc:
            self.val[e] += 1
            sem = self.sem[e]
            self.prog[e].append(lambda eng: fn(eng).then_inc(sem, 1))
            ev = (e, self.val[e])
        else:
            self.prog[e].append(fn)
            ev = (e, self.val[e] + 1)
        self._commit(ev, reads, writes)

    def dma(self, q, out, in_, reads=(), writes=()):
        k = self.ring[q][self.ring_i[q]]
        self.ring_i[q] = (self.ring_i[q] + 1) % len(self.ring[q])
        if self.val[k]:
            self._wait(q, (k, self.val[k]))
        self._deps(q, reads, writes)
        sem = self.sem[k]
        self.prog[q].append(lambda eng: eng.dma_start(out=out, in_=in_).then_inc(sem, 16))
        self.val[k] += 16
        ev = (k, self.val[k])
        self._commit(ev, reads, writes)

    def fence(self, res_list):
        for e in self.E:
            for r in res_list:
                if r.w is not None:
                    self._wait(e, r.w)

    def barrier(self):
        for e in self.E:
            for k in ["pe", "act", "dve", "pool"]:
                if k != e and self.val[k]:
                    self._wait(e, (k, self.val[k]))

    def finish(self, q="sp"):
        for k in self.ring["sp"] + self.ring["pool"]:
            if self.val[k]:
                self._wait(q, (k, self.val[k]))
        prog = self.prog
        with self.nc.Block() as block:
            @block.sync
            def _(eng):
                for t in prog["sp"]:
                    t(eng)

            @block.tensor
            def _(eng):
                for t in prog["pe"]:
                    t(eng)

            @block.scalar
            def _(eng):
                for t in prog["act"]:
                    t(eng)

            @block.vector
            def _(eng):
                for t in prog["dve"]:
                    t(eng)

            @block.gpsimd
            def _(eng):
                for t in prog["pool"]:
                    t(eng)


class Arena:
    def __init__(self, nc, name, nelem, dt):
        self.t = nc.sbuf_tensor(name, [128, nelem], dt).__enter__()
        self.n = nelem
        self.off = 0
        self.top = nelem

    def reset(self):
        self.off = 0

    def alloc(self, n, parts=128):
        o = self.off
        n2 = (n + 15) // 16 * 16
        self.off += n2
        assert self.off <= self.top, (self.off, self.top)
        return self.t[0:parts, o:o + n]

    def alloc_top(self, n, parts=128):
        n2 = (n + 15) // 16 * 16
        self.top -= n2
        assert self.off <= self.top, (self.off, self.top)
        return self.t[0:parts, self.top:self.top + n]


def ffn_groups():
    return [[0, 1, 2], [3, 4, 5], [6, 7, 8], [9, 10]]


def weight_schedule(dr):
    L = []

    def in_tile(tag, w, pieces):
        wv = w.rearrange("(k p) c -> p k c", p=128)
        ps = []
        o = 0
        for (c0, n) in pieces:
            ps.append((o, o + n, wv[:, :, c0:c0 + n]))
            o += n
        L.append((tag, "in", ps))

    def out_tile(tag, w, r0):
        wv = w[r0:r0 + 256, :].rearrange("(r p) c -> p r c", p=128)
        L.append((tag, "out", [(0, 1024, wv)]))

    ew, eo, ow, oo = dr["even_w_in"], dr["even_w_out"], dr["odd_w_in"], dr["odd_w_out"]
    for cp in range(2):
        for i, nm in enumerate("abc"):
            in_tile((nm, cp), ew, [(i * 512 + cp * 256, 256)])
    out_tile(("eo", 0), eo, 0)
    out_tile(("eo", 1), eo, 256)
    for hh in range(4):
        in_tile(("qf", hh), ew, [(1536 + hh * 128, 128), (2048 + hh * 128, 128)])
    for i in range(2):
        in_tile(("hi", i), ew, [(2560 + i * 256, 256)])
    for i in range(2):
        in_tile(("hg", i), ew, [(3072 + i * 256, 256)])
    out_tile(("eo", 2), eo, 512)
    out_tile(("eo", 3), eo, 768)

    def ffn(l):
        wi, wo = dr["ffn_w_in"][l], dr["ffn_w_out"][l]
        for grp in ffn_groups():
            for jp in grp:
                in_tile(("ffg", l, jp), wi, [(jp * 256, 256)])
                in_tile(("ffu", l, jp), wi, [(FF + jp * 256, 256)])
            for jp in grp:
                out_tile(("ffo", l, jp), wo, jp * 256)

    ffn(0)
    for hh in range(4):
        in_tile(("rq", hh), ow, [(hh * 256, 256)])
        in_tile(("rk", hh), ow, [(1024 + hh * 256, 256)])
        in_tile(("rv", hh, 0), ow, [(2048 + hh * 512, 256)])
        in_tile(("rv", hh, 1), ow, [(2048 + hh * 512 + 256, 256)])
        in_tile(("rg", hh, 0), ow, [(4096 + hh * 512, 256)])
        in_tile(("rg", hh, 1), ow, [(4096 + hh * 512 + 256, 256)])
        out_tile(("ro", hh, 0), oo, hh * 512)
        out_tile(("ro", hh, 1), oo, hh * 512 + 256)
    ffn(1)
    return L


class WStream:
    def __init__(self, S, nc, lst, NS):
        self.S = S
        self.NS = NS
        self.lst = lst
        self.ring = nc.sbuf_tensor("wring", [128, NS, 2048], BF16).__enter__()
        self.res = [Res() for _ in range(NS)]
        self.issued = 0
        self.consumed = 0
        self.released = [False] * len(lst)

    def view(self, i):
        s = i % self.NS
        if self.lst[i][1] == "in":
            return self.ring[:, s, :].rearrange("p (k c) -> p k c", k=8)
        return self.ring[:, s, :].rearrange("p (r c) -> p r c", r=2)

    def pump(self):
        while self.issued < len(self.lst):
            i = self.issued
            if i >= self.NS and not self.released[i - self.NS]:
                break
            v = self.view(i)
            for (c0, c1, src) in self.lst[i][2]:
                self.S.dma("pool", v[:, :, c0:c1], src, writes=[self.res[i % self.NS]])
            self.issued += 1

    def next(self, tag):
        i = self.consumed
        assert self.lst[i][0] == tag, (self.lst[i][0], tag)
        self.pump()
        assert self.issued > i, ("weight slot not available", tag)
        self.consumed += 1
        return i, self.view(i), self.res[i % self.NS]

    def done(self, i):
        self.released[i] = True
        self.pump()


def build_program(SL, NS=8):
    T = SL // 128
    GS = min(512, SL)
    NG = SL // GS
    TPG = GS // 128
    NCH = SL // 64
    CPG = GS // 64
    nc = bass.Bass("TRN2", target_bir_lowering=False)
    dr = {}

    def din(name, shape, dt=F32):
        dr[name] = nc.dram_tensor(name, shape, dt, kind="ExternalInput").ap()

    din("x", [SL, D])
    din("positions", [1, SL], I32)
    din("norm_mix_w", [128, 2 * KD])
    din("norm_ffn_w", [128, 2 * KD])
    din("norm_final_w", [1, D])
    din("even_w_in", [D, EVEN_IN])
    din("conv_w", [128, 12])
    din("hgrn_lb_logits", [128, 8])
    din("hgrn_norm_w", [1, 512])
    din("even_w_out", [1024, D])
    din("odd_w_in", [D, ODD_IN])
    din("ret_norm_w", [1, 2048])
    din("odd_w_out", [2048, D])
    din("ffn_w_in", [2, D, 2 * FF])
    din("ffn_w_out", [2, FF, D])
    din("c_ident", [128, 128])
    din("c_mask", [128, 128])
    din("c_smask", [128, 512])
    din("c_invf", [128, 1])
    din("c_rmask", [128, 512])
    din("c_rvec", [128, 12])
    out_d = nc.dram_tensor("out", [SL, D], F32, kind="ExternalOutput").ap()
    rot_d = nc.dram_tensor("rot_scratch", [2, 128, SL], F32).ap()
    r_rot = [Res(), Res()]

    S = Sched(nc)

    def sbt(name, shape, dt):
        return nc.sbuf_tensor(name, shape, dt).__enter__()

    h = sbt("h", [128, T, D], F32)
    r_h = [Res() for _ in range(T)]
    hnT = sbt("hnT", [128, KD, SL], BF16)
    r_hn = [Res() for _ in range(T)]
    W = WStream(S, nc, weight_schedule(dr), NS)
    ident_f = sbt("ident_f", [128, 128], F32)
    ident = sbt("ident", [128, 128], BF16)
    r_id = Res()
    cmask = sbt("cmask", [128, 128], F32)
    r_cm = Res()
    smask = sbt("smask", [128, 512], F32)
    r_sm = Res()
    nmw = sbt("nmw", [128, 2, KD], F32)
    nfw = sbt("nfw", [128, 2, KD], F32)
    r_nw = Res()
    small = sbt("small", [128, 256], F32)
    r_small = Res()
    AR = Arena(nc, "arena", 17600, F32)

    class _FA:
        @staticmethod
        def alloc(n, parts=128):
            return AR.alloc(n, parts)

    class _BA:
        @staticmethod
        def alloc(n, parts=128):
            m = (n + 1) // 2
            return AR.alloc(m, parts).bitcast(BF16)[:, 0:n]

    FA = _FA
    BA = _BA

    pm = [nc.psum_tensor("pm%d" % i, [128, 512], F32).__enter__() for i in range(8)]
    r_pm = [Res() for _ in range(8)]
    ptr = [pm[6 + i][:, :].bitcast(BF16) for i in range(2)]
    r_ptr = [r_pm[6], r_pm[7]]
    cnt = {"pm": 0, "ptr": 0}

    def bank():
        i = cnt["pm"] % 6
        cnt["pm"] += 1
        return pm[i], r_pm[i]

    def tbank():
        i = cnt["ptr"] % 2
        cnt["ptr"] += 1
        return ptr[i], r_ptr[i]

    def MM(out, lhsT, rhs, start, stop, reads, writes, inc):
        S.op("pe", lambda e: e.matmul(out, lhsT=lhsT, rhs=rhs, start=start, stop=stop), reads, writes, inc)

    def PROJ(out, lhs_list, rhs_list, reads, writes):
        n = len(lhs_list)
        for i in range(n):
            MM(out, lhs_list[i], rhs_list[i], i == 0, i == n - 1, reads, writes, i == n - 1)

    def TR(out, in_, np_, reads, writes, inc=True):
        idn = ident[0:np_, 0:np_]
        S.op("pe", lambda e: e.transpose(out=out, in_=in_, identity=idn), list(reads) + [r_id], writes, inc)

    def ACT(out, in_, func, reads, writes, scale=1.0, accum_out=None, bias=None):
        if bias is not None:
            S.op("act", lambda e: e.activation(out=out, in_=in_, func=func, scale=scale, bias=bias), reads, writes)
        elif accum_out is None:
            S.op("act", lambda e: e.activation(out=out, in_=in_, func=func, scale=scale), reads, writes)
        else:
            S.op("act", lambda e: e.activation(out=out, in_=in_, func=func, scale=scale, accum_out=accum_out),
                 reads, writes)

    def TT(eng, out, in0, in1, op, reads, writes):
        S.op(eng, lambda e: e.tensor_tensor(out=out, in0=in0, in1=in1, op=op), reads, writes)

    def TS(eng, out, in0, s1, s2, op0, op1, reads, writes):
        if s2 is None:
            S.op(eng, lambda e: e.tensor_scalar(out=out, in0=in0, scalar1=s1, scalar2=None, op0=op0), reads, writes)
        else:
            S.op(eng, lambda e: e.tensor_scalar(out=out, in0=in0, scalar1=s1, scalar2=s2, op0=op0, op1=op1),
                 reads, writes)

    def STT(out, in0, scalar, in1, op0, op1, reads, writes):
        S.op("dve", lambda e: e.scalar_tensor_tensor(out=out, in0=in0, scalar=scalar, in1=in1, op0=op0, op1=op1),
             reads, writes)

    def CP(eng, out, in_, reads, writes):
        S.op(eng, lambda e: e.tensor_copy(out=out, in_=in_), reads, writes)

    def RECIP(out, in_, reads, writes):
        S.op("dve", lambda e: e.reciprocal(out=out, in_=in_), reads, writes)

    def phase():
        S.barrier()
        AR.reset()

    S.dma("sp", ident_f[:], dr["c_ident"], writes=[r_id])
    S.dma("sp", cmask[:], dr["c_mask"], writes=[r_cm])
    S.dma("sp", smask[:], dr["c_smask"], writes=[r_sm])
    S.dma("sp", nmw[:].rearrange("p l k -> p (l k)"), dr["norm_mix_w"], writes=[r_nw])
    S.dma("sp", nfw[:].rearrange("p l k -> p (l k)"), dr["norm_ffn_w"], writes=[r_nw])
    S.dma("sp", small[:, 0:12], dr["conv_w"], writes=[r_small])
    S.dma("sp", small[:, 16:24], dr["hgrn_lb_logits"], writes=[r_small])
    S.dma("sp", small[:, 32:33], dr["c_invf"], writes=[r_small])
    xv = dr["x"].rearrange("(t p) d -> p t d", p=128)
    for t in range(T):
        S.dma("sp", h[:, t, :], xv[:, t, :], writes=[r_h[t]])
    CP("dve", ident[:], ident_f[:], [r_id], [r_id])
    TT("dve", small[:, 36:40], small[:, 16:20], small[:, 20:24], ALU.subtract, [r_small], [r_small])
    ACT(small[:, 40:44], small[:, 36:40], AF.Sigmoid, [r_small], [r_small])
    ACT(small[:, 44:48], small[:, 36:40], AF.Sigmoid, [r_small], [r_small], scale=-1.0)
    TS("dve", small[:, 48:52], small[:, 44:48], -1.0, None, ALU.mult, None, [r_small], [r_small])
    LB, OML, NOML = 40, 44, 48
    S.op("dve", lambda e: e.memset(small[:, 60:61], EPS), [r_small], [r_small])
    eps_col = small[:, 60:61]

    junk = sbt("junk", [128, D], BF16)
    r_junk = Res()
    xn = [sbt("xn%d" % i, [128, D], BF16) for i in range(2)]
    r_xn = [Res(), Res()]
    nst = sbt("nst", [128, 64], F32)
    r_nst = [Res() for _ in range(T)]

    def norm_gen(t, wcol, final=None):
        rn = r_nst[t]
        ACT(junk[:], h[:, t, :], AF.Square, [r_h[t]], [r_junk, rn], accum_out=nst[:, t:t + 1])
        yield
        TS("dve", nst[:, 16 + t:17 + t], nst[:, t:t + 1], 1.0 / D, EPS, ALU.mult, ALU.add, [rn], [rn])
        yield
        ACT(nst[:, 16 + t:17 + t], nst[:, 16 + t:17 + t], AF.Sqrt, [rn], [rn])
        yield
        RECIP(nst[:, 32 + t:33 + t], nst[:, 16 + t:17 + t], [rn], [rn])
        if final is not None:
            o_, ro = final["ot"][t % 2], final["r_ot"][t % 2]
            STT(o_, h[:, t, :], nst[:, 32 + t:33 + t], final["fw"], ALU.mult, ALU.mult,
                [r_h[t], rn, final["r_fw"]], [ro])
            yield
            S.dma("sp", final["ov"][:, t, :], o_, reads=[ro])
            return
        yield
        x_, rx = xn[t % 2], r_xn[t % 2]
        ACT(x_[:], h[:, t, :], AF.Copy, [r_h[t], rn], [rx], scale=nst[:, 32 + t:33 + t])
        yield
        wb = wcol.unsqueeze(2).to_broadcast([128, KD, 128])
        pt, rpt = tbank()
        for k in range(KD):
            TR(pt[:, k * 128:(k + 1) * 128], x_[:, k * 128:(k + 1) * 128], 128, [rx], [rpt], inc=(k == KD - 1))
        yield
        TT("dve", hnT[:, :, t * 128:(t + 1) * 128], pt[:].rearrange("p (k c) -> p k c", k=KD), wb, ALU.mult,
           [rpt, r_nw], [r_hn[t]])

    class NormPipe:
        def __init__(self, wcol, final=None):
            self.wcol = wcol
            self.final = final
            self.q = []
            self.fresh = []

        def push(self, t):
            g_ = norm_gen(t, self.wcol, self.final)
            next(g_)
            self.fresh.append(g_)

        def tick(self):
            for g_ in list(self.q):
                try:
                    next(g_)
                except StopIteration:
                    self.q.remove(g_)
            self.q += self.fresh
            self.fresh = []

        def flush(self):
            while self.q or self.fresh:
                self.tick()

    def hn_reads(g):
        return r_hn[g * TPG:(g + 1) * TPG]

    def out_proj_add(lhs_fn, nk, rhs_fn, reads, after_tile=None):
        for t in range(T):
            for half in range(2):
                p, rp = bank()
                PROJ(p[:, :], [lhs_fn(j, t) for j in range(nk)], [rhs_fn(j, half) for j in range(nk)], reads(t), [rp])
                hs = h[:, t, half * 512:(half + 1) * 512]
                TT("dve", hs, p[:, :], hs, ALU.add, [rp, r_h[t]], [r_h[t]])
            if after_tile is not None:
                after_tile.tick()
                after_tile.push(t)
        if after_tile is not None:
            after_tile.flush()

    np0 = NormPipe(nmw[:, 0, :])
    for t in range(T):
        np0.tick()
        np0.push(t)
    np0.flush()
    phase()
    TC = min(512, SL)
    g_cos = AR.alloc_top(TC)
    g_sin = AR.alloc_top(TC)
    tA = AR.alloc_top(TC)
    tB = AR.alloc_top(TC)
    r_tab = Res()
    RT = [r_tab]
    pos_i = tB.bitcast(I32)
    rot_stores = []

    def table_piece(ci):
        cl = slice(ci * TC, (ci + 1) * TC)
        S.dma("sp", pos_i, dr["positions"][:, cl].partition_broadcast(128), writes=RT)
        ACT(tA, pos_i, AF.Copy, RT + [r_small], RT, scale=small[:, 32:33])
        TS("dve", pos_i, tA, 1.0 / TWO_PI, None, ALU.mult, None, RT, RT)
        CP("dve", g_cos, pos_i, RT, RT)
        STT(tA, g_cos, -CW1, tA, ALU.mult, ALU.add, RT, RT)
        STT(tA, g_cos, -CW2, tA, ALU.mult, ALU.add, RT, RT)

        def wrap(buf):
            TS("dve", tB, buf, math.pi, -TWO_PI, ALU.is_gt, ALU.mult, RT, RT)
            TT("dve", buf, buf, tB, ALU.add, RT, RT)
            TS("dve", tB, buf, -math.pi, TWO_PI, ALU.is_lt, ALU.mult, RT, RT)
            TT("dve", buf, buf, tB, ALU.add, RT, RT)

        wrap(tA)
        ACT(g_sin, tA, AF.Sin, RT, RT)
        TS("dve", tA, tA, math.pi / 2, None, ALU.add, None, RT, RT)
        wrap(tA)
        ACT(g_cos, tA, AF.Sin, RT, RT)
        r0, r1 = Res(), Res()
        S.dma("sp", rot_d[0][:, cl], g_cos, reads=RT, writes=[r0])
        S.dma("sp", rot_d[1][:, cl], g_sin, reads=RT, writes=[r1])
        r_tab.r[r0.w[0]] = r0.w[1]
        r_tab.r[r1.w[0]] = r1.w[1]
        rot_stores.extend([r0, r1])

    n_pieces = SL // TC
    mixT = BA.alloc(4 * SL).rearrange("p (c s) -> p c s", c=4)
    r_mix = [Res() for _ in range(NG)]
    ub = [FA.alloc(SL + 2), FA.alloc(SL + 2)]
    r_ub = [Res(), Res()]
    a_sb = [FA.alloc(GS), FA.alloc(GS)]
    r_asb = [Res(), Res()]
    tcv = [FA.alloc(GS), FA.alloc(GS)]
    r_tcv = [Res(), Res()]
    for i in range(2):
        S.op("dve", lambda e, i=i: e.memset(ub[i][:, 0:2], 0.0), [], [r_ub[i]])
    it = 0
    for cp in range(2):
        ia, wa, ra = W.next(("a", cp))
        ib, wb_, rb = W.next(("b", cp))
        ic, wc, rc = W.next(("c", cp))
        for ci in range(2):
            cc = cp * 2 + ci
            u, ru = ub[cc % 2], r_ub[cc % 2]
            cs = slice(ci * 128, (ci + 1) * 128)
            for g in range(NG):
                gsl = slice(g * GS, (g + 1) * GS)
                rhs = [hnT[:, k, gsl] for k in range(KD)]
                pa, rpa = bank()
                PROJ(pa[:, 0:GS], [wa[:, k, cs] for k in range(KD)], rhs, [ra] + hn_reads(g), [rpa])
                pc, rpc = bank()
                PROJ(pc[:, 0:GS], [wc[:, k, cs] for k in range(KD)], rhs, [rc] + hn_reads(g), [rpc])
                pb, rpb = bank()
                PROJ(pb[:, 0:GS], [wb_[:, k, cs] for k in range(KD)], rhs, [rb] + hn_reads(g), [rpb])
                asb, rasb = a_sb[it % 2], r_asb[it % 2]
                tc_, rtc = tcv[it % 2], r_tcv[it % 2]
                it += 1
                ACT(asb[:, :], pa[:, 0:GS], AF.Copy, [rpa], [rasb])
                TT("dve", u[:, 2 + g * GS:2 + (g + 1) * GS], pc[:, 0:GS], asb[:, :], ALU.mult, [rpc, rasb], [ru])
                ACT(tc_[:, :], u[:, 2 + g * GS:2 + (g + 1) * GS], AF.Copy, [ru, r_small], [rtc],
                    scale=small[:, 8 + cc:9 + cc])
                STT(tc_[:, :], u[:, 1 + g * GS:1 + (g + 1) * GS], small[:, 4 + cc:5 + cc], tc_[:, :], ALU.mult, ALU.add,
                    [ru, rtc, r_small], [rtc])
                STT(tc_[:, :], u[:, g * GS:(g + 1) * GS], small[:, cc:cc + 1], tc_[:, :], ALU.mult, ALU.add,
                    [ru, rtc, r_small], [rtc])
                TT("dve", mixT[:, cc, gsl], pb[:, 0:GS], tc_[:, :], ALU.mult, [rpb, rtc], [r_mix[g]])
            if cc < n_pieces:
                table_piece(cc)
        W.done(ia)
        W.done(ib)
        W.done(ic)
    i0, wo0, ro0 = W.next(("eo", 0))
    i1, wo1, ro1 = W.next(("eo", 1))
    wos = [wo0, wo1]
    out_proj_add(lambda j, t: mixT[:, j, t * 128:(t + 1) * 128], 4,
                 lambda j, half: wos[j // 2][:, j % 2, half * 512:(half + 1) * 512],
                 lambda t: [r_mix[t // TPG], ro0, ro1])
    W.done(i0)
    W.done(i1)

    for ci in range(4, n_pieces):
        table_piece(ci)
    S.fence(rot_stores)
    AR.top = AR.n
    phase()
    gwn = FA.alloc(512, parts=64)
    r_gwn = Res()
    S.dma("sp", gwn, dr["hgrn_norm_w"].partition_broadcast(64), writes=[r_gwn])
    qt = BA.alloc(4 * SL).rearrange("p (h s) -> p h s", h=4)
    kt = BA.alloc(4 * SL).rearrange("p (h s) -> p h s", h=4)
    r_qk = [[Res() for _ in range(NG)] for _ in range(4)]
    Eh = FA.alloc(4 * NCH).rearrange("p (h c) -> p h c", h=4)
    r_E = Res()
    markA = AR.off
    tmpA = [{n: FA.alloc(GS) for n in ["sg", "qs", "kk", "bb", "eb", "enb", "sqt"]} for _ in range(2)]
    r_tmpA = [{n: Res() for n in ["sg", "qs", "kk", "bb", "eb", "enb", "sqt"]} for _ in range(2)]
    qf_tiles = {}

    def stageA(it, hh, g):
        tm, rt = tmpA[it % 2], r_tmpA[it % 2]
        sg, qs, kk, bb, eb, enb, sqt = (tm[n] for n in ["sg", "qs", "kk", "bb", "eb", "enb", "sqt"])
        if g == 0:
            qf_tiles[hh] = W.next(("qf", hh))
        iqf, wqf, rqf = qf_tiles[hh]
        lbc = small[:, LB + hh:LB + hh + 1]
        omlc = small[:, OML + hh:OML + hh + 1]
        nomlc = small[:, NOML + hh:NOML + hh + 1]
        gsl = slice(g * GS, (g + 1) * GS)
        rhs = [hnT[:, k, gsl] for k in range(KD)]
        pq, rpq = bank()
        PROJ(pq[:, 0:GS], [wqf[:, k, 0:128] for k in range(KD)], rhs, [rqf] + hn_reads(g), [rpq])
        pf, rpf = bank()
        PROJ(pf[:, 0:GS], [wqf[:, k, 128:256] for k in range(KD)], rhs, [rqf] + hn_reads(g), [rpf])
        if g == NG - 1:
            W.done(iqf)
        ACT(sg[:, :], pf[:, 0:GS], AF.Sigmoid, [rpf], [rt["sg"]])
        ACT(sqt[:, :], pq[:, 0:GS], AF.Sigmoid, [rpq], [rt["sqt"]])
        TT("dve", qs[:, :], pq[:, 0:GS], sqt[:, :], ALU.mult, [rpq, rt["sqt"]], [rt["qs"]])
        ACT(kk[:, :], sg[:, :], AF.Identity, [rt["sg"], r_small], [rt["kk"]], scale=nomlc, bias=omlc)
        ACT(sg[:, :], sg[:, :], AF.Identity, [rt["sg"], r_small], [rt["sg"]], scale=omlc, bias=lbc)
        yield
        S.op("dve", lambda e: e.tensor_tensor_scan(out=eb[:, :], data0=smask[:, 0:GS], data1=sg[:, :], initial=0.0,
                                                   op0=ALU.max, op1=ALU.mult),
             [rt["sg"], r_sm], [rt["eb"]])
        RECIP(enb[:, :], eb[:, :], [rt["eb"]], [rt["enb"]])
        TT("pool", qt[:, hh, gsl], qs[:, :], eb[:, :], ALU.mult, [rt["qs"], rt["eb"]], [r_qk[hh][g]])
        TT("pool", kt[:, hh, gsl], kk[:, :], enb[:, :], ALU.mult, [rt["kk"], rt["enb"]], [r_qk[hh][g]])
        CP("dve", Eh[:, hh, g * CPG:(g + 1) * CPG], eb[:, :].rearrange("p (c j) -> p c j", j=64)[:, :, 63],
           [rt["eb"]], [r_E])
        yield

    gensA = [stageA(i, i // NG, i % NG) for i in range(4 * NG)]
    for i in range(4 * NG + 1):
        if i < 4 * NG:
            next(gensA[i])
        if i >= 1:
            next(gensA[i - 1])
    S.barrier()
    AR.off = markA

    whi = [W.next(("hi", i)) for i in range(2)]
    whg = [W.next(("hg", i)) for i in range(2)]
    weo = [W.next(("eo", 2 + i)) for i in range(2)]
    V_c = [BA.alloc(512, parts=64) for _ in range(2)]
    r_Vc = [Res(), Res()]
    gs_c = FA.alloc(512, parts=64)
    r_gsc = Res()
    GW_t = [BA.alloc(1024, parts=64).rearrange("p (c v) -> p c v", c=2) for _ in range(3)]
    r_GW = [[Res(), Res()] for _ in range(3)]
    o_t = [FA.alloc(1024, parts=64).rearrange("p (c v) -> p c v", c=2) for _ in range(2)]
    r_ot = [[Res(), Res()], [Res(), Res()]]
    ktc = [BA.alloc(512, parts=64) for _ in range(2)]
    r_ktc = [Res(), Res()]
    sTc = [BA.alloc(256, parts=64) for _ in range(2)]
    r_sTc = [Res(), Res()]
    U4 = FA.alloc(512).rearrange("p (h v) -> p h v", h=4)
    r_U4 = [Res() for _ in range(4)]
    st4 = [BA.alloc(512).rearrange("p (h v) -> p h v", h=4) for _ in range(2)]
    r_st4 = [[Res() for _ in range(4)] for _ in range(2)]
    hstt = [FA.alloc(32, parts=64) for _ in range(2)]
    r_hstt = [Res(), Res()]
    mixt = [BA.alloc(512).rearrange("p (h s) -> p h s", h=4) for _ in range(2)]
    r_mixt = [Res(), Res()]
    pkT = pm[6][0:64, :].bitcast(BF16)
    pyT = pm[7][:, :].bitcast(BF16)
    mask4 = cmask[0:64, 0:64].unsqueeze(1).to_broadcast([64, 4, 64])

    def chunk_gen(c):
        csl = slice(c * 64, (c + 1) * 64)
        g = c // CPG
        t = c // 2
        cj = c % 2
        lhs = [hnT[:, k, csl] for k in range(KD)]
        for i in range(2):
            PROJ(pm[0][0:64, i * 256:(i + 1) * 256], lhs, [whi[i][1][:, k, :] for k in range(KD)],
                 [whi[i][2], r_hn[t]], [r_pm[0]])
        Vc, rVc = V_c[c % 2], r_Vc[c % 2]
        ACT(Vc, pm[0][0:64, :], AF.Copy, [r_pm[0]], [rVc])
        for i in range(2):
            PROJ(pm[1][0:64, i * 256:(i + 1) * 256], lhs, [whg[i][1][:, k, :] for k in range(KD)],
                 [whg[i][2], r_hn[t]], [r_pm[1]])
        ACT(gs_c, pm[1][0:64, :], AF.Silu, [r_pm[1]], [r_gsc])
        GW, rGW = GW_t[t % 3], r_GW[t % 3]
        TT("pool", GW[:, cj, :], gs_c, gwn, ALU.mult, [r_gsc, r_gwn], [rGW[cj]])
        for hh in range(4):
            TR(pkT[:, hh * 128:(hh + 1) * 128], kt[:, hh, csl], 128, [r_qk[hh][g]], [r_pm[6]], inc=(hh == 3))
        kc, rkc = ktc[c % 2], r_ktc[c % 2]
        CP("dve", kc, pkT[:, 0:512], [r_pm[6]], [rkc])
        for hh in range(4):
            MM(pm[2][0:64, hh * 64:(hh + 1) * 64], kt[:, hh, csl], qt[:, hh, csl], True, True, [r_qk[hh][g]], [r_pm[2]],
               hh == 3)
        sT, rsT = sTc[c % 2], r_sTc[c % 2]
        TT("dve", sT.rearrange("p (h i) -> p h i", h=4), pm[2][0:64, 0:256].rearrange("p (h i) -> p h i", h=4), mask4,
           ALU.mult, [r_pm[2], r_cm], [rsT])
        yield
        for hh in range(4):
            hs_ = slice(hh * 128, (hh + 1) * 128)
            MM(pm[3][0:64, hs_], sT[:, hh * 64:(hh + 1) * 64], Vc[:, hs_], True, c == 0, [rsT, rVc], [r_pm[3]],
               c == 0 and hh == 3)
            if c > 0:
                MM(pm[3][0:64, hs_], qt[:, hh, csl], st4[(c - 1) % 2][:, hh, :], False, True,
                   [r_qk[hh][g], r_st4[(c - 1) % 2][hh]], [r_pm[3]], hh == 3)
        ot_, rot_ = o_t[t % 2], r_ot[t % 2]
        ACT(ot_[:, cj, :], pm[3][0:64, :], AF.Copy, [r_pm[3]], [rot_[cj]])
        if c < NCH - 1:
            for hh in range(4):
                hs_ = slice(hh * 128, (hh + 1) * 128)
                MM(pm[4][:, hs_], kc[:, hs_], Vc[:, hs_], True, True, [rkc, rVc], [r_pm[4]], hh == 3)
            for hh in range(4):
                hs_ = slice(hh * 128, (hh + 1) * 128)
                if c == 0:
                    CP("dve", U4[:, hh, :], pm[4][:, hs_], [r_pm[4]], [r_U4[hh]])
                else:
                    STT(U4[:, hh, :], U4[:, hh, :], Eh[:, hh, c - 1:c], pm[4][:, hs_], ALU.mult, ALU.add,
                        [r_U4[hh], r_E, r_pm[4]], [r_U4[hh]])
                TS("dve", st4[c % 2][:, hh, :], U4[:, hh, :], Eh[:, hh, c:c + 1], None, ALU.mult, None,
                   [r_U4[hh], r_E], [r_st4[c % 2][hh]])
        yield

    def tile_gen(t):
        ot_, rot_ = o_t[t % 2], r_ot[t % 2]
        GW, rGW = GW_t[t % 3], r_GW[t % 3]
        hs, rhs_ = hstt[t % 2], r_hstt[t % 2]
        for cj in range(2):
            for hh in range(4):
                ACT(junk[0:64, 0:128], ot_[:, cj, hh * 128:(hh + 1) * 128], AF.Square, [rot_[cj]], [r_junk, rhs_],
                    accum_out=hs[:, cj * 4 + hh:cj * 4 + hh + 1])
        TS("dve", hs[:, 8:16], hs[:, 0:8], 1.0 / 128, EPS, ALU.mult, ALU.add, [rhs_], [rhs_])
        ACT(hs[:, 8:16], hs[:, 8:16], AF.Sqrt, [rhs_], [rhs_])
        RECIP(hs[:, 16:24], hs[:, 8:16], [rhs_], [rhs_])
        yield
        o4 = ot_[:, :, :].rearrange("p c (h v) -> p (c h) v", h=4)
        rstd_b = hs[:, 16:24].unsqueeze(2).to_broadcast([64, 8, 128])
        TT("dve", o4, o4, rstd_b, ALU.mult, [rot_[0], rot_[1], rhs_], [rot_[0], rot_[1]])
        TT("pool", GW[:, :, :], ot_[:, :, :], GW[:, :, :], ALU.mult, [rot_[0], rot_[1], rGW[0], rGW[1]], [rGW[0], rGW[1]])
        yield
        for cj in range(2):
            for hh in range(4):
                TR(pyT[:, hh * 128 + cj * 64:hh * 128 + (cj + 1) * 64], GW[:, cj, hh * 128:(hh + 1) * 128], 64,
                   [rGW[cj]], [r_pm[7]], inc=(cj == 1 and hh == 3))
        mx, rmx = mixt[t % 2], r_mixt[t % 2]
        ACT(mx, pyT[:, 0:512].rearrange("p (h s) -> p h s", h=4), AF.Copy, [r_pm[7]], [rmx])
        yield
        for half in range(2):
            PROJ(pm[5][:, :], [mx[:, j, :] for j in range(4)],
                 [weo[j // 2][1][:, j % 2, half * 512:(half + 1) * 512] for j in range(4)],
                 [rmx, weo[0][2], weo[1][2]], [r_pm[5]])
            hsl = h[:, t, half * 512:(half + 1) * 512]
            TT("dve", hsl, pm[5][:, :], hsl, ALU.add, [r_pm[5], r_h[t]], [r_h[t]])
            yield

    npH = NormPipe(nfw[:, 0, :])
    cg = [chunk_gen(c) for c in range(NCH)]
    tg = [tile_gen(t) for t in range(T)]
    tg_calls = [0] * T

    def tstep(t, n):
        if 0 <= t < T:
            assert tg_calls[t] == n, (t, tg_calls[t], n)
            next(tg[t])
            tg_calls[t] += 1

    for i in range(NCH + 8):
        if i < NCH:
            next(cg[i])
        if 0 <= i - 1 < NCH:
            next(cg[i - 1])
        if i % 2 == 0:
            tstep((i - 2) // 2, 0)
            tstep((i - 4) // 2, 2)
            tstep((i - 6) // 2, 4)
        else:
            tstep((i - 3) // 2, 1)
            tstep((i - 5) // 2, 3)
    assert all(n == 5 for n in tg_calls), tg_calls
    for w_ in whi + whg + weo:
        W.done(w_[0])
    for t in range(T):
        npH.tick()
        npH.push(t)
    npH.flush()

    def ffn(l, tail_setup, tail_tile):
        phase()
        tail_setup()
        actT = [BA.alloc(6 * SL).rearrange("p (c s) -> p c s", c=6) for _ in range(2)]
        r_act = [[[Res() for _ in range(NG)] for _ in range(6)] for _ in range(2)]
        sgt = [FA.alloc(GS), FA.alloc(GS)]
        r_sgt = [Res(), Res()]
        it = 0
        for gi, grp in enumerate(ffn_groups()):
            aT, rA = actT[gi % 2], r_act[gi % 2]
            for li, jp in enumerate(grp):
                ig_, wg, rg = W.next(("ffg", l, jp))
                iu_, wu, ru = W.next(("ffu", l, jp))
                for ci in range(2):
                    jj = li * 2 + ci
                    cs = slice(ci * 128, (ci + 1) * 128)
                    for g in range(NG):
                        gsl = slice(g * GS, (g + 1) * GS)
                        rhs = [hnT[:, k, gsl] for k in range(KD)]
                        pg, rpg = bank()
                        PROJ(pg[:, 0:GS], [wg[:, k, cs] for k in range(KD)], rhs, [rg] + hn_reads(g), [rpg])
                        pu, rpu = bank()
                        PROJ(pu[:, 0:GS], [wu[:, k, cs] for k in range(KD)], rhs, [ru] + hn_reads(g), [rpu])
                        st_, rst = sgt[it % 2], r_sgt[it % 2]
                        it += 1
                        ACT(st_[:, :], pg[:, 0:GS], AF.Silu, [rpg], [rst])
                        TT("dve", aT[:, jj, gsl], pu[:, 0:GS], st_[:, :], ALU.mult, [rpu, rst], [rA[jj][g]])
                W.done(ig_)
                W.done(iu_)
            wts = [W.next(("ffo", l, jp)) for jp in grp]
            nk = 2 * len(grp)
            out_proj_add(lambda j, t: aT[:, j, t * 128:(t + 1) * 128], nk,
                         lambda j, half: wts[j // 2][1][:, j % 2, half * 512:(half + 1) * 512],
                         lambda t: [rA[j][t // TPG] for j in range(nk)] + [w_[2] for w_ in wts],
                         after_tile=(tail_tile if gi == len(ffn_groups()) - 1 else None))
            for w_ in wts:
                W.done(w_[0])

    rot_sb = {}

    def rot_prefetch():
        rot_sb["cos"] = AR.alloc_top(SL)
        rot_sb["sin"] = AR.alloc_top(SL)
        rot_sb["r"] = Res()
        S.dma("sp", rot_sb["cos"], rot_d[0], reads=rot_stores, writes=[rot_sb["r"]])
        S.dma("sp", rot_sb["sin"], rot_d[1], reads=rot_stores, writes=[rot_sb["r"]])

    ffn(0, rot_prefetch, NormPipe(nmw[:, 1, :]))

    phase()
    cosT, sinT, r_cs = rot_sb["cos"], rot_sb["sin"], rot_sb["r"]
    rmask = FA.alloc(512)
    rvec = FA.alloc(16)
    r_rc = Res()
    S.dma("sp", rmask, dr["c_rmask"], writes=[r_rc])
    S.dma("sp", rvec[:, 0:12], dr["c_rvec"], writes=[r_rc])
    rnw = FA.alloc(512)
    r_rnw = Res()
    qT = BA.alloc(2 * SL).rearrange("p (a s) -> p a s", a=2)
    kT = BA.alloc(2 * SL).rearrange("p (a s) -> p a s", a=2)
    r_q = [Res() for _ in range(NG)]
    r_k = [Res() for _ in range(NG)]
    HG = GS // 2
    tt = [[FA.alloc(HG) for _ in range(4)] for _ in range(2)]
    r_tt = [[Res() for _ in range(4)] for _ in range(2)]
    V_sb = [BA.alloc(512), BA.alloc(512)]
    r_V = [Res(), Res()]
    gsb = [FA.alloc(512), FA.alloc(512)]
    r_gs = [Res(), Res()]
    ki_sb = [BA.alloc(256), BA.alloc(256)]
    r_ki = [Res(), Res()]
    sT2 = [BA.alloc(128), BA.alloc(128)]
    r_sT2 = [Res(), Res()]
    Ur = FA.alloc(1024).rearrange("p (a v) -> p a v", a=2)
    r_Ur = Res()
    stb = [BA.alloc(1024).rearrange("p (a v) -> p a v", a=2) for _ in range(2)]
    r_stb2 = [Res(), Res()]
    bnst = [FA.alloc(16), FA.alloc(16)]
    r_bn = [Res(), Res()]
    y1 = FA.alloc(512)
    r_y1 = Res()
    y_sb = [BA.alloc(512) for _ in range(3)]
    r_ysb = [Res() for _ in range(3)]
    yT = [BA.alloc(512).rearrange("p (j c) -> p j c", j=4) for _ in range(2)]
    r_yT = [Res(), Res()]
    rot_it = [0]
    hb_ps = pm[6][:, 0:128]
    hb_pt2 = pm[6][:, 256:512].bitcast(BF16)
    hb_pt = pm[7][:, 0:128].bitcast(BF16)
    r_hb = [r_pm[6], r_pm[7], r_pm[6]]
    for hh in range(4):
        gC = (1.0 - 2.0 ** (-5 - hh)) ** 128
        S.dma("sp", rnw, dr["ret_norm_w"][:, hh * 512:(hh + 1) * 512].partition_broadcast(128), writes=[r_rnw])
        mask_h = rmask[:, hh * 128:(hh + 1) * 128]
        c_h = rvec[:, hh:hh + 1]
        c2_h = rvec[:, 4 + hh:5 + hh]
        dk_h = rvec[:, 8 + hh:9 + hh]
        for (tag, dst, rdst) in (("rq", qT, r_q), ("rk", kT, r_k)):
            iw, ww, rw = W.next((tag, hh))
            for g in range(NG):
                gsl = slice(g * GS, (g + 1) * GS)
                rhs = [hnT[:, k, gsl] for k in range(KD)]
                pA, rpA = bank()
                PROJ(pA[:, 0:GS], [ww[:, k, 0:128] for k in range(KD)], rhs, [rw] + hn_reads(g), [rpA])
                pB, rpB = bank()
                PROJ(pB[:, 0:GS], [ww[:, k, 128:256] for k in range(KD)], rhs, [rw] + hn_reads(g), [rpB])
                for hf in range(2):
                    cl = slice(hf * HG, (hf + 1) * HG)
                    al = slice(g * GS + hf * HG, g * GS + (hf + 1) * HG)
                    t_, rt_ = tt[rot_it[0] % 2], r_tt[rot_it[0] % 2]
                    rot_it[0] += 1
                    TT("dve", t_[0], pA[:, cl], cosT[:, al], ALU.mult, [rpA, r_cs], [rt_[0]])
                    TT("dve", t_[1], pB[:, cl], sinT[:, al], ALU.mult, [rpB, r_cs], [rt_[1]])
                    TT("dve", t_[2], pA[:, cl], sinT[:, al], ALU.mult, [rpA, r_cs], [rt_[2]])
                    TT("dve", t_[3], pB[:, cl], cosT[:, al], ALU.mult, [rpB, r_cs], [rt_[3]])
                    TT("pool", dst[:, 0, al], t_[0], t_[1], ALU.subtract, [rt_[0], rt_[1]], [rdst[g]])
                    TT("pool", dst[:, 1, al], t_[2], t_[3], ALU.add, [rt_[2], rt_[3]], [rdst[g]])
            W.done(iw)
        wv = [W.next(("rv", hh, i)) for i in range(2)]
        wg_ = [W.next(("rg", hh, i)) for i in range(2)]
        wo_ = [W.next(("ro", hh, i)) for i in range(2)]

        def ret_tile(t):
            tsl = slice(t * 128, (t + 1) * 128)
            g = t // TPG
            lhs = [hnT[:, k, tsl] for k in range(KD)]
            pv, rpv = pm[0], r_pm[0]
            for i in range(2):
                PROJ(pv[:, i * 256:(i + 1) * 256], lhs, [wv[i][1][:, k, :] for k in range(KD)], [wv[i][2], r_hn[t]], [rpv])
            V, rV = V_sb[t % 2], r_V[t % 2]
            ACT(V, pv[:, :], AF.Copy, [rpv], [rV])
            pg, rpg = pm[1], r_pm[1]
            for i in range(2):
                PROJ(pg[:, i * 256:(i + 1) * 256], lhs, [wg_[i][1][:, k, :] for k in range(KD)], [wg_[i][2], r_hn[t]], [rpg])
            gs_, rgs = gsb[t % 2], r_gs[t % 2]
            ACT(gs_, pg[:, :], AF.Silu, [rpg], [rgs])
            TT("pool", gs_, gs_, rnw, ALU.mult, [rgs, r_rnw], [rgs])
            pt, rpt = hb_pt, r_hb[1]
            for a in range(2):
                TR(pt[:, a * 128:(a + 1) * 128], kT[:, a, tsl], 128, [r_k[g]], [rpt], inc=(a == 1))
            ki, rki = ki_sb[t % 2], r_ki[t % 2]
            TS("dve", ki, pt[:, 0:256], dk_h, None, ALU.mult, None, [rpt, r_rc], [rki])
            ps_, rps = hb_ps, r_hb[0]
            PROJ(ps_[:, 0:128], [kT[:, a, tsl] for a in range(2)], [qT[:, a, tsl] for a in range(2)],
                 [r_k[g], r_q[g]], [rps])
            sT, rsT = sT2[t % 2], r_sT2[t % 2]
            TT("dve", sT, ps_[:, 0:128], mask_h, ALU.mult, [rps, r_rc], [rsT])
            yield
            po, rpo = pm[2], r_pm[2]
            MM(po[:, :], sT, V, True, t == 0, [rsT, rV], [rpo], t == 0)
            if t > 0:
                sb_, rsb = stb[(t - 1) % 2], r_stb2[(t - 1) % 2]
                for a in range(2):
                    MM(po[:, :], qT[:, a, tsl], sb_[:, a, :], False, a == 1, [r_q[g], rsb], [rpo], a == 1)
            pkvs = []
            if t < T - 1:
                for a in range(2):
                    pkv, rpkv = pm[3 + a], r_pm[3 + a]
                    MM(pkv[:, :], ki[:, a * 128:(a + 1) * 128], V, True, True, [rki, rV], [rpkv], True)
                    pkvs.append((pkv, rpkv))
            bn, rbn = bnst[t % 2], r_bn[t % 2]
            S.op("dve", lambda e: e.bn_stats(out=bn[:, 0:6], in_=po[:, :]), [rpo], [rbn])
            S.op("dve", lambda e: e.bn_aggr(out=bn[:, 8:10], in_=bn[:, 0:6]), [rbn], [rbn])
            TS("dve", bn[:, 10:11], bn[:, 9:10], c2_h, EPS, ALU.mult, ALU.add, [rbn, r_rc], [rbn])
            ACT(bn[:, 10:11], bn[:, 10:11], AF.Sqrt, [rbn], [rbn])
            for a, (pkv, rpkv) in enumerate(pkvs):
                if t == 0:
                    CP("dve", Ur[:, a, :], pkv[:, :], [rpkv], [r_Ur])
                else:
                    STT(Ur[:, a, :], Ur[:, a, :], gC, pkv[:, :], ALU.mult, ALU.add, [r_Ur, rpkv], [r_Ur])
            if pkvs:
                TS("pool", stb[t % 2][:, :, :], Ur[:, :, :], gC, 1.0, ALU.mult, ALU.mult, [r_Ur], [r_stb2[t % 2]])
            RECIP(bn[:, 11:12], bn[:, 10:11], [rbn], [rbn])
            TS("dve", bn[:, 11:12], bn[:, 11:12], c_h, None, ALU.mult, None, [rbn, r_rc], [rbn])
            TS("dve", y1, po[:, :], bn[:, 8:9], bn[:, 11:12], ALU.subtract, ALU.mult, [rpo, rbn], [r_y1])
            ys, rys = y_sb[t % 3], r_ysb[t % 3]
            TT("pool", ys, y1, gs_, ALU.mult, [r_y1, rgs], [rys])
            yield
            pt2, rpt2 = hb_pt2, r_hb[2]
            for j in range(4):
                TR(pt2[:, j * 128:(j + 1) * 128], ys[:, j * 128:(j + 1) * 128], 128, [rys], [rpt2], inc=(j == 3))
            yT_, ryT = yT[t % 2], r_yT[t % 2]
            ACT(yT_, pt2[:, 0:512].rearrange("p (j c) -> p j c", j=4), AF.Copy, [rpt2], [ryT])
            yield
            for half in range(2):
                p, rp = pm[5], r_pm[5]
                PROJ(p[:, :], [yT_[:, j, :] for j in range(4)],
                     [wo_[j // 2][1][:, j % 2, half * 512:(half + 1) * 512] for j in range(4)],
                     [ryT, wo_[0][2], wo_[1][2]], [rp])
                hs = h[:, t, half * 512:(half + 1) * 512]
                TT("dve", hs, p[:, :], hs, ALU.add, [rp, r_h[t]], [r_h[t]])
                yield

        npR = NormPipe(nfw[:, 1, :])
        gens = [ret_tile(t) for t in range(T)]
        stage_of = [0] * T
        for i in range(T + 3):
            for (lag, st_) in ((3, 2), (0, 0), (3, 3), (1, 1), (3, 4)):
                t = i - lag
                if 0 <= t < T:
                    assert stage_of[t] == st_, (t, stage_of[t], st_)
                    next(gens[t])
                    stage_of[t] += 1
        for w_ in wv + wg_ + wo_:
            W.done(w_[0])
    for t in range(T):
        npR.tick()
        npR.push(t)
    npR.flush()
    AR.top = AR.n

    fin = {}
    ov = out_d.rearrange("(t p) d -> p t d", p=128)

    fin["ov"] = ov

    def final_setup():
        fin["fw"] = FA.alloc(D)
        fin["r_fw"] = Res()
        S.dma("sp", fin["fw"], dr["norm_final_w"].partition_broadcast(128), writes=[fin["r_fw"]])
        fin["ot"] = [FA.alloc(D), FA.alloc(D)]
        fin["r_ot"] = [Res(), Res()]

    ffn(1, final_setup, NormPipe(None, final=fin))
    assert W.consumed == len(W.lst)
    S.finish("sp")
    return nc


def make_consts():
    c = {}
    c["c_ident"] = np.eye(128, dtype=np.float32)
    j = np.arange(128)
    c["c_mask"] = (j[:, None] <= j[None, :]).astype(np.float32)
    sm = np.zeros((128, 512), dtype=np.float32)
    sm[:, ::64] = 1.0
    c["c_smask"] = sm
    c["c_invf"] = (10000.0 ** (-np.arange(128, dtype=np.float64) / 128)).astype(np.float32).reshape(128, 1)
    rm = np.zeros((128, 4, 128))
    rv = np.zeros((128, 12))
    for hh in range(4):
        gam = 1.0 - 2.0 ** (-5 - hh)
        kinv = gam ** (-(j + 1.0)) * (256 ** -0.5)
        rm[:, hh, :] = (j[:, None] <= j[None, :]) * kinv[:, None]
        rv[:, hh] = gam ** (j + 1.0)
        rv[:, 4 + hh] = gam ** (2 * (j + 1.0))
        rv[:, 8 + hh] = kinv
    c["c_rmask"] = rm.reshape(128, 512).astype(np.float32)
    c["c_rvec"] = rv.astype(np.float32)
    return c


def make_in_maps(inputs, n, SL):
    f = lambda a: np.ascontiguousarray(np.asarray(a))
    shared = {
        "norm_mix_w": f(f(inputs["norm_mix_w"]).reshape(2, KD, 128).transpose(2, 0, 1)).reshape(128, 2 * KD),
        "norm_ffn_w": f(f(inputs["norm_ffn_w"]).reshape(2, KD, 128).transpose(2, 0, 1)).reshape(128, 2 * KD),
        "norm_final_w": f(inputs["norm_final_w"]).reshape(1, D),
        "even_w_in": f(inputs["even_w_in"])[0],
        "conv_w": f(f(inputs["conv_w"])[0].reshape(3, 4, 128).transpose(2, 0, 1)).reshape(128, 12),
        "hgrn_lb_logits": f(f(inputs["hgrn_lb_logits"]).reshape(2, 4, 128).transpose(2, 0, 1)).reshape(128, 8),
        "hgrn_norm_w": f(inputs["hgrn_norm_w"]).reshape(1, 512),
        "even_w_out": f(inputs["even_w_out"])[0],
        "odd_w_in": f(inputs["odd_w_in"])[0],
        "ret_norm_w": f(inputs["ret_norm_w"]).reshape(1, 2048),
        "odd_w_out": f(inputs["odd_w_out"])[0],
        "ffn_w_in": f(inputs["ffn_w_in"]),
        "ffn_w_out": f(inputs["ffn_w_out"]),
    }
    shared.update(make_consts())
    x = f(inputs["x"])
    pos = f(inputs["positions"]).astype(np.int32)
    maps = []
    for b in range(n):
        m = dict(shared)
        m["x"] = np.ascontiguousarray(x[b])
        m["positions"] = np.ascontiguousarray(pos[b].reshape(1, SL))
        maps.append(m)
    return maps


_CACHE = {}


def kernel(**inputs):
    x = np.asarray(inputs["x"])
    B, SL, _ = x.shape
    if SL not in _CACHE:
        _CACHE[SL] = build_program(SL)
    nc = _CACHE[SL]
    maps = make_in_maps(inputs, B, SL)
    res = run_bass_kernel_spmd(nc, maps, core_ids=list(range(B)))
    return np.stack([np.asarray(r["out"]) for r in res.results], axis=0).astype(np.float32)
```
